# Optimizing a Trainium2 kernel written in Bass

```python
import jax, jax.numpy as jnp
from jax import lax
import numpy as np

D_MODEL = 4096
BATCH = 1
SEQ = 16384
DEPTH = 1

PLE_DIM = 256
HG_DK = 128
HG_DV = 128
HG_HEADS = (D_MODEL // 2) // HG_DK
HG_W = HG_HEADS * HG_DK
HG_VW = HG_HEADS * HG_DV
GD_DK = 128
GD_DV = 128
GD_HEADS = (D_MODEL // 2) // GD_DV
GD_KW = GD_HEADS * GD_DK
GD_VW = GD_HEADS * GD_DV
GD_QKV = 2 * GD_KW + GD_VW
GD_CONV = 5
CHUNK = 64
MIX_WIDTH = HG_VW + GD_VW
IN_SPLITS = (HG_W, HG_W, HG_W, HG_VW, HG_VW, GD_QKV, 2 * GD_HEADS, 2 * GD_HEADS, GD_VW)
IN_WIDTH = sum(IN_SPLITS)
PEER_HEADS = 8
PEER_NKEYS = 128
PEER_KEY_DIM = 128
PEER_QUERY_DIM = 2 * PEER_KEY_DIM
PEER_TOPK = 16
PEER_N_EXPERTS = PEER_NKEYS * PEER_NKEYS
PEER_BLOCK = 64
EPS = 1e-6

kernel_name = "hymba_hgrn2_gdn_peer_encoder"


def rmsnorm(x, w):
    xf = x.astype(jnp.float32)
    y = xf * lax.rsqrt(jnp.mean(xf * xf, axis=-1, keepdims=True) + EPS)
    return (y * w.astype(jnp.float32)).astype(x.dtype)


def l2norm(t):
    return t * lax.rsqrt(jnp.sum(t * t, axis=-1, keepdims=True) + EPS)


def heads(t, n):
    b, l, w = t.shape
    return t.reshape(b, l, n, w // n).transpose(0, 2, 1, 3)


def flip_seq(t):
    return jnp.flip(t, axis=2)


def to_chunks(t):
    b, h, l = t.shape[:3]
    t = t.reshape(b, h, l // CHUNK, CHUNK, *t.shape[3:])
    return jnp.moveaxis(t, 2, 0)


def from_chunks(t):
    n, b, h, c = t.shape[:4]
    return jnp.moveaxis(t, 0, 2).reshape(b, h, n * c, *t.shape[4:])


def centred_depthwise_conv(x, w):
    k, c = w.shape
    pad = (k - 1) // 2
    return lax.conv_general_dilated(
        x, w[:, None, :].astype(x.dtype), window_strides=(1,),
        padding=[(pad, k - 1 - pad)], dimension_numbers=('NWC', 'WIO', 'NWC'),
        feature_group_count=c)


def hgrn2_scan(q, k, v, log_f):
    b_, h_, _, dk = q.shape
    dv = v.shape[-1]
    qc, kc, vc = to_chunks(q), to_chunks(k), to_chunks(v)
    bcum = jnp.cumsum(to_chunks(log_f), axis=-2)
    incl = jnp.tril(jnp.ones((CHUNK, CHUNK), bool))

    def step(S, inp):
        qi, ki, vi, bi = inp
        rel = jnp.exp(jnp.where(incl[:, :, None],
                                bi[..., :, None, :] - bi[..., None, :, :], -jnp.inf))
        attn = jnp.sum(qi[..., :, None, :] * ki[..., None, :, :] * rel, axis=-1)
        b_last = bi[..., -1:, :]
        o = (jnp.einsum('bhts,bhsv->bhtv', attn, vi)
             + jnp.einsum('bhtk,bhkv->bhtv', qi * jnp.exp(bi), S))
        S = (S * jnp.swapaxes(jnp.exp(b_last), -1, -2)
             + jnp.einsum('bhsk,bhsv->bhkv', ki * jnp.exp(b_last - bi), vi))
        return S, o

    S0 = jnp.zeros((b_, h_, dk, dv), jnp.float32)
    _, o = lax.scan(step, S0, (qc, kc, vc, bcum))
    return from_chunks(o)


def gdn_scan(q, k, v, g, beta):
    b_, h_, _, dk = q.shape
    dv = v.shape[-1]
    qc, kc, vc = to_chunks(q), to_chunks(k), to_chunks(v)
    gam = jnp.cumsum(to_chunks(g), axis=-1)
    bc = to_chunks(beta)
    incl = jnp.tril(jnp.ones((CHUNK, CHUNK), bool))
    strict = jnp.tril(jnp.ones((CHUNK, CHUNK), bool), -1)
    rel = jnp.exp(jnp.where(incl, gam[..., :, None] - gam[..., None, :], -jnp.inf))
    kk = jnp.einsum('nbhid,nbhjd->nbhij', kc, kc)
    a_mat = jnp.where(strict, bc[..., :, None] * kk * rel, 0.0) + jnp.eye(CHUNK, dtype=jnp.float32)
    rhs = jnp.concatenate([vc * bc[..., None], kc * (bc * jnp.exp(gam))[..., None]], axis=-1)
    sol = lax.linalg.triangular_solve(a_mat, rhs, left_side=True, lower=True, unit_diagonal=True)
    u_c, w_c = sol[..., :dv], sol[..., dv:]
    qk = jnp.einsum('nbhid,nbhjd->nbhij', qc, kc) * rel
    q_dec = qc * jnp.exp(gam)[..., None]
    k_dec = kc * jnp.exp(gam[..., -1:] - gam)[..., None]
    g_last = jnp.exp(gam[..., -1])

    def step(S, inp):
        ui, wi, qki, qdi, kdi, gli = inp
        v_new = ui - jnp.einsum('bhck,bhkv->bhcv', wi, S)
        o = jnp.einsum('bhck,bhkv->bhcv', qdi, S) + jnp.einsum('bhij,bhjv->bhiv', qki, v_new)
        S = S * gli[..., None, None] + jnp.einsum('bhck,bhcv->bhkv', kdi, v_new)
        return S, o

    S0 = jnp.zeros((b_, h_, dk, dv), jnp.float32)
    _, o = lax.scan(step, S0, (u_c, w_c, qk, q_dec, k_dec, g_last))
    return from_chunks(o)


def gated_head_norm(o, z, w):
    b_, h_, l_, dv = o.shape
    y = rmsnorm(jnp.swapaxes(o, 1, 2), w) * jax.nn.silu(z.astype(jnp.float32)).reshape(b_, l_, h_, dv)
    return y.reshape(b_, l_, h_ * dv).astype(z.dtype)


def peer(xn, w_query, sub_keys, expert_u, expert_v):
    b_, l_, d_ = xn.shape
    t_ = b_ * l_
    xt = xn.reshape(t_, d_)
    qry = (xt @ w_query).reshape(t_, PEER_HEADS, 2, PEER_KEY_DIM)
    scores = jnp.einsum('thpd,hpkd->thpk', qry, sub_keys).astype(jnp.float32)
    s_top, i_top = lax.top_k(scores, PEER_TOPK)
    cand = (s_top[..., 0, :, None] + s_top[..., 1, None, :]).reshape(t_, PEER_HEADS, PEER_TOPK * PEER_TOPK)
    c_top, c_idx = lax.top_k(cand, PEER_TOPK)
    i1 = jnp.take_along_axis(i_top[..., 0, :], c_idx // PEER_TOPK, axis=-1)
    i2 = jnp.take_along_axis(i_top[..., 1, :], c_idx % PEER_TOPK, axis=-1)
    idx = (i1 * PEER_NKEYS + i2).reshape(t_, PEER_HEADS * PEER_TOPK)
    gates = jax.nn.softmax(c_top, axis=-1).reshape(t_, PEER_HEADS * PEER_TOPK)
    nb = t_ // PEER_BLOCK

    def block(args):
        xc, ic, gc = args
        u = expert_u[ic]
        hid = jnp.einsum('cd,ced->ce', xc, u).astype(jnp.float32)
        act = (jax.nn.gelu(hid, approximate=False) * gc).astype(xc.dtype)
        return jnp.einsum('ce,ced->cd', act, expert_v[ic])

    out = lax.map(block, (xt.reshape(nb, PEER_BLOCK, d_),
                          idx.reshape(nb, PEER_BLOCK, -1),
                          gates.reshape(nb, PEER_BLOCK, -1)))
    return out.reshape(b_, l_, d_)


def setup_inputs(seed: int = 0) -> dict:
    key = jax.random.key(seed)
    ks = jax.random.split(key, 24)
    f32 = jnp.float32
    nrm = lambda k, s, sc: jax.random.normal(k, s, f32) * sc
    gain = lambda k, s: 1.0 + 0.02 * jax.random.normal(k, s, f32)
    dt = jnp.exp(jax.random.uniform(ks[7], (DEPTH, 2, GD_HEADS), f32, np.log(1e-3), np.log(1e-1)))
    return {
        "x": nrm(ks[0], (BATCH, SEQ, D_MODEL), 1.0),
        "p": nrm(ks[1], (DEPTH, BATCH, SEQ, PLE_DIM), 1.0),
        "attn_norm_w": gain(ks[2], (DEPTH, D_MODEL)),
        "w_in": nrm(ks[3], (DEPTH, D_MODEL, IN_WIDTH), D_MODEL ** -0.5),
        "hg_lower_bound": nrm(ks[4], (DEPTH + 1, 2, HG_W), 0.1),
        "gd_conv_w": nrm(ks[5], (DEPTH, GD_CONV, GD_QKV), GD_CONV ** -0.5),
        "gd_A_log": jnp.log(jax.random.uniform(ks[6], (DEPTH, 2, GD_HEADS), f32, 1.0, 16.0)),
        "gd_dt_bias": dt + jnp.log(-jnp.expm1(-dt)),
        "hg_out_norm_w": gain(ks[8], (DEPTH, HG_DV)),
        "gd_out_norm_w": gain(ks[9], (DEPTH, GD_DV)),
        "w_out": nrm(ks[10], (DEPTH, MIX_WIDTH, D_MODEL), MIX_WIDTH ** -0.5),
        "ffn_norm_w": gain(ks[11], (DEPTH, D_MODEL)),
        "peer_w_query": nrm(ks[12], (DEPTH, D_MODEL, PEER_HEADS * PEER_QUERY_DIM), D_MODEL ** -0.5),
        "peer_sub_keys": nrm(ks[13], (DEPTH, PEER_HEADS, 2, PEER_NKEYS, PEER_KEY_DIM), PEER_KEY_DIM ** -0.5),
        "peer_u": nrm(ks[14], (DEPTH, PEER_N_EXPERTS, D_MODEL), D_MODEL ** -0.5),
        "peer_v": nrm(ks[15], (DEPTH, PEER_N_EXPERTS, D_MODEL), PEER_HEADS ** -0.5),
        "ple_norm_w": gain(ks[16], (DEPTH, D_MODEL)),
        "ple_w_gate": nrm(ks[17], (DEPTH, D_MODEL, D_MODEL), D_MODEL ** -0.5),
        "ple_w_proj": nrm(ks[18], (DEPTH, PLE_DIM, D_MODEL), PLE_DIM ** -0.5),
        "ple_post_norm_w": gain(ks[19], (DEPTH, D_MODEL)),
        "final_norm_w": gain(ks[20], (D_MODEL,)),
    }


def reference(x, p, attn_norm_w, w_in, hg_lower_bound, gd_conv_w, gd_A_log, gd_dt_bias,
              hg_out_norm_w, gd_out_norm_w, w_out, ffn_norm_w, peer_w_query, peer_sub_keys,
              peer_u, peer_v, ple_norm_w, ple_w_gate, ple_w_proj, ple_post_norm_w, final_norm_w):
    f32 = jnp.float32
    split_pts = []
    acc = 0
    for w in IN_SPLITS[:-1]:
        acc += w
        split_pts.append(acc)
    lb_all = jnp.cumsum(jax.nn.softmax(hg_lower_bound.astype(f32), axis=0), axis=0)
    h = x
    for i in range(DEPTH):
        xn = rmsnorm(h, attn_norm_w[i])
        proj = xn @ w_in[i]
        hq, hf_fw, hf_bw, hi, hz, gqkv, ga, gb, gz = jnp.split(proj, split_pts, axis=-1)

        lb = lb_all[i]
        q_a = heads(jax.nn.silu(hq.astype(f32)), HG_HEADS) * (HG_DK ** -0.5)
        v_a = heads(hi.astype(f32), HG_HEADS)
        zf_fw, zf_bw = hf_fw.astype(f32), hf_bw.astype(f32)
        logf_fw = jnp.logaddexp(jnp.log(lb[0]), jnp.log1p(-lb[0]) + jax.nn.log_sigmoid(zf_fw))
        logf_bw = jnp.logaddexp(jnp.log(lb[1]), jnp.log1p(-lb[1]) + jax.nn.log_sigmoid(zf_bw))
        k_fw = (1.0 - lb[0]) * jax.nn.sigmoid(-zf_fw)
        k_bw = (1.0 - lb[1]) * jax.nn.sigmoid(-zf_bw)
        o_a = (hgrn2_scan(q_a, heads(k_fw, HG_HEADS), v_a, heads(logf_fw, HG_HEADS))
               + flip_seq(hgrn2_scan(flip_seq(q_a), flip_seq(heads(k_bw, HG_HEADS)),
                                     flip_seq(v_a), flip_seq(heads(logf_bw, HG_HEADS)))))
        out_a = gated_head_norm(o_a, hz, hg_out_norm_w[i])

        qkv = jax.nn.silu(centred_depthwise_conv(gqkv, gd_conv_w[i]).astype(f32))
        q_b = l2norm(heads(qkv[..., :GD_KW], GD_HEADS)) * (GD_DK ** -0.5)
        k_b = l2norm(heads(qkv[..., GD_KW:2 * GD_KW], GD_HEADS))
        v_b = heads(qkv[..., 2 * GD_KW:], GD_HEADS)
        bl = ga.shape[:2]
        a_in = ga.astype(f32).reshape(*bl, 2, GD_HEADS)
        dec = -jnp.exp(gd_A_log[i].astype(f32)) * jax.nn.softplus(a_in + gd_dt_bias[i].astype(f32))
        dec = jnp.transpose(dec, (2, 0, 3, 1))
        beta = jnp.transpose(jax.nn.sigmoid(gb.astype(f32).reshape(*bl, 2, GD_HEADS)), (2, 0, 3, 1))
        o_b = (gdn_scan(q_b, k_b, v_b, dec[0], beta[0])
               + flip_seq(gdn_scan(flip_seq(q_b), flip_seq(k_b), flip_seq(v_b),
                                   flip_seq(dec[1]), flip_seq(beta[1]))))
        out_b = gated_head_norm(o_b, gz, gd_out_norm_w[i])

        mix = jnp.concatenate([out_a, out_b], axis=-1).astype(h.dtype) @ w_out[i]
        h = h + mix

        h = h + peer(rmsnorm(h, ffn_norm_w[i]), peer_w_query[i], peer_sub_keys[i], peer_u[i], peer_v[i])

        gate = jax.nn.sigmoid((rmsnorm(h, ple_norm_w[i]) @ ple_w_gate[i]).astype(f32))
        emb = rmsnorm(p[i].astype(h.dtype) @ ple_w_proj[i], ple_post_norm_w[i]).astype(f32)
        h = h + (gate * emb).astype(h.dtype)
    return rmsnorm(h, final_norm_w)
```

```python
import numpy as np, os
import concourse.bass as bass
import concourse.mybir as mybir

F32 = mybir.dt.float32
BF16 = mybir.dt.bfloat16
ALU = mybir.AluOpType
AF = mybir.ActivationFunctionType
AX = mybir.AxisListType

SEM_CAP = int(os.environ.get("SEM_CAP", "20000"))


class Buf:
    def __init__(self, ap, name):
        self.ap = ap
        self.name = name
        self.writers = []
        self.readers = []

    def __getitem__(self, idx):
        return self.ap[idx]


class Op:
    __slots__ = ("eng", "fn", "deps", "id", "is_dma", "signals", "sig_idx", "dma_sem", "dma_val", "stage", "phys")

    def __init__(self, eng, fn, deps, id_, is_dma):
        self.eng, self.fn, self.deps, self.id, self.is_dma = eng, fn, deps, id_, is_dma
        self.signals = False
        self.sig_idx = -1
        self.dma_sem = None
        self.dma_val = 0
        self.stage = 0
        self.phys = -1


class Emitter:
    ENGS = ("pe", "act", "dve", "pool", "sp")

    def __init__(self, nc, same_engine_sync=True):
        self.nc = nc
        self.ops = []
        self.same_engine_sync = same_engine_sync
        self.dma_groups = {}

    def op(self, eng, fn, reads=(), writes=(), dma_key=None):
        deps = set()
        for b in reads:
            deps.update(b.writers)
        for b in writes:
            deps.update(b.writers)
            deps.update(b.readers)
        oid = len(self.ops)
        o = Op(eng, fn, deps, oid, dma_key is not None)
        self.ops.append(o)
        if dma_key is not None:
            self.dma_groups.setdefault(dma_key, []).append(oid)
            o.dma_sem = dma_key
        for b in reads:
            b.readers.append(oid)
        for b in writes:
            b.writers = [oid]
            b.readers = []
        return oid

    def op_partial_write(self, eng, fn, reads=(), writes=(), dma_key=None):
        deps = set()
        for b in reads:
            deps.update(b.writers)
        for b in writes:
            deps.update(b.readers)
        oid = len(self.ops)
        o = Op(eng, fn, deps, oid, dma_key is not None)
        self.ops.append(o)
        if dma_key is not None:
            self.dma_groups.setdefault(dma_key, []).append(oid)
            o.dma_sem = dma_key
        for b in reads:
            b.readers.append(oid)
        for b in writes:
            b.writers = b.writers + [oid]
        return oid

    def emit(self, final_wait_ops=()):
        nc = self.nc
        ops = self.ops
        for o in ops:
            for d in o.deps:
                t = ops[d]
                if t.is_dma:
                    continue
                if t.eng != o.eng or self.same_engine_sync or o.is_dma:
                    t.signals = True
        nsig = {e: 0 for e in self.ENGS}
        for o in ops:
            if o.is_dma:
                continue
            if o.signals:
                o.sig_idx = nsig[o.eng]
                nsig[o.eng] += 1
        phys = {}
        per_stage = {}
        for o in ops:
            if o.is_dma:
                k = (o.stage, o.dma_sem)
                if k not in phys:
                    idx = per_stage.get(o.stage, 0)
                    phys[k] = idx
                    per_stage[o.stage] = idx + 1
                o.phys = phys[k]
        nphys = max(per_stage.values()) if per_stage else 0
        cum = [0] * nphys
        for o in ops:
            if o.is_dma:
                cum[o.phys] += 16
                o.dma_val = cum[o.phys]
        esems = {}
        for e in self.ENGS:
            n_ep = (nsig[e] + SEM_CAP - 1) // SEM_CAP
            esems[e] = [nc.alloc_semaphore(name=f"e_{e}_{i}") for i in range(n_ep)]
        dsems = [nc.alloc_semaphore(name=f"d_{i}") for i in range(nphys)]
        self.n_sems = sum(len(v) for v in esems.values()) + len(dsems)

        def target(t):
            if t.is_dma:
                return (dsems[t.phys], t.dma_val, ("d", t.phys))
            ep, v = divmod(t.sig_idx, SEM_CAP)
            return (esems[t.eng][ep], v + 1, ("e", t.eng, ep))

        progs = {e: [] for e in self.ENGS}
        seen = {e: {} for e in self.ENGS}
        for o in ops:
            waits = {}
            for d in o.deps:
                t = ops[d]
                if (not t.is_dma) and t.eng == o.eng and not (self.same_engine_sync or o.is_dma):
                    continue
                sem, val, k = target(t)
                if seen[o.eng].get(k, 0) >= val:
                    continue
                if k not in waits or waits[k][1] < val:
                    waits[k] = (sem, val)
            for k, (sem, val) in waits.items():
                seen[o.eng][k] = val
            progs[o.eng].append((list(waits.values()), o))
        fin = []
        for oid in final_wait_ops:
            sem, val, k = target(ops[oid])
            fin.append((sem, val))

        handles = {}

        def run(ename, eng):
            for waits, o in progs[ename]:
                for sem, val in waits:
                    eng.wait_ge(sem, val)
                ins = o.fn(eng)
                if o.is_dma:
                    ins.then_inc(dsems[o.phys], 16)
                elif o.signals:
                    ep = o.sig_idx // SEM_CAP
                    ins.then_inc(esems[ename][ep], 1)
            if ename == "sp":
                for sem, val in fin:
                    eng.wait_ge(sem, val)

        with nc.Block() as block:
            @block.tensor
            def _(e):
                run("pe", e)

            @block.scalar
            def _(e):
                run("act", e)

            @block.vector
            def _(e):
                run("dve", e)

            @block.gpsimd
            def _(e):
                run("pool", e)

            @block.sync
            def _(e):
                run("sp", e)
        self.counts = {e: len(progs[e]) for e in self.ENGS}


def _barrier(self):
    last = {}
    for o in self.ops:
        if o.is_dma:
            last[("d", o.dma_sem)] = o.id
        else:
            last[("e", o.eng)] = o.id
    self._bar_deps = set(last.values())
    self._stage = getattr(self, "_stage", 0) + 1


_orig_op = Emitter.op
_orig_opp = Emitter.op_partial_write


def _op(self, eng, fn, reads=(), writes=(), dma_key=None):
    oid = _orig_op(self, eng, fn, reads, writes, dma_key)
    self.ops[oid].stage = getattr(self, "_stage", 0)
    bd = getattr(self, "_bar_deps", None)
    if bd:
        self.ops[oid].deps.update(bd)
    return oid


def _opp(self, eng, fn, reads=(), writes=(), dma_key=None):
    oid = _orig_opp(self, eng, fn, reads, writes, dma_key)
    self.ops[oid].stage = getattr(self, "_stage", 0)
    bd = getattr(self, "_bar_deps", None)
    if bd:
        self.ops[oid].deps.update(bd)
    return oid


Emitter.barrier = _barrier
Emitter.op = _op
Emitter.op_partial_write = _opp


import numpy as np, os
from contextlib import ExitStack
import concourse.bass as bass
import concourse.mybir as mybir

D = 4096
KC = D // 128
NTOK = 1544
NFEAT = 768
EPS = 1e-6
C_ID, C_ONES, C_HMF, C_HMB, C_CIND, C_HKF, C_HKB, C_UF, C_UB, C_MBI, C_MBIT, C_MBS, C_MBST, C_END = (
    0, 128, 256, 384, 512, 520, 584, 648, 776, 904, 1032, 1160, 1288, 1416)


def make_consts():
    c = np.zeros((128, C_END), np.float32)
    p = np.arange(128)[:, None]
    f = np.arange(128)[None, :]
    same = (p // 64) == (f // 64)
    c[:, C_ID:C_ID + 128] = (p == f)
    c[:, C_ONES:C_ONES + 128] = 1.0
    c[:, C_HMF:C_HMF + 128] = same & (p > f)
    c[:, C_HMB:C_HMB + 128] = same & (p < f)
    c[:, C_CIND] = (np.arange(128) < 64)
    c[:, C_CIND + 1] = (np.arange(128) >= 64)
    f64 = np.arange(64)[None, :]
    c[:, C_HKF:C_HKF + 64] = (p <= f64)
    c[:, C_HKB:C_HKB + 64] = (p >= f64)
    c[:, C_UF:C_UF + 128] = (p <= f)
    c[:, C_UB:C_UB + 128] = (p >= f)
    NEG = -30000.0
    c[:, C_MBI:C_MBI + 128] = np.where(p >= f, 0.0, NEG)
    c[:, C_MBIT:C_MBIT + 128] = np.where(f >= p, 0.0, NEG)
    c[:, C_MBS:C_MBS + 128] = np.where(p > f, 0.0, NEG)
    c[:, C_MBST:C_MBST + 128] = np.where(f > p, 0.0, NEG)
    return c


class _Stop(Exception):
    pass


def build_p1(L, debug=False, stop_after=99):
    nc = bass.Bass("TRN2", target_bir_lowering=False)
    NB = L // 512
    NT = L // 128
    NCH = L // 64
    dt_in = lambda n, s: nc.dram_tensor(n, s, F32, kind="ExternalInput").ap()
    xT = dt_in("xT", [D, L])
    w_tok = dt_in("w_tok", [D, NTOK])
    w_feat = dt_in("w_feat", [D, NFEAT])
    consts_d = dt_in("consts", [128, C_END])
    anw = dt_in("anw", [128, KC])
    lbp = dt_in("lbp", [1, 2 * 2 * 2 * 128])
    convw = dt_in("convw", [6, 128, 5])
    gdp = dt_in("gdp", [1, 8])
    nws = dt_in("nws", [1, 256])
    outT = nc.dram_tensor("outT", [512, L], F32, kind="ExternalOutput").ap()

    em = Emitter(nc)
    cnt = [0]

    def dram(name, shape, dt):
        return nc.dram_tensor(name, shape, dt, kind="Internal").ap()

    regs = {}

    def R(name, key):
        k = (name, key)
        if k not in regs:
            regs[k] = Buf(None, f"{name}_{key}")
        return regs[k]

    def sbt(es, name, shape, dt):
        h = es.enter_context(nc.sbuf_tensor(name, shape, dt))
        return Buf(h.ap() if hasattr(h, "ap") else h, name)

    def pst(es, name, shape, dt):
        h = es.enter_context(nc.psum_tensor(name, shape, dt))
        return Buf(h.ap() if hasattr(h, "ap") else h, name)

    def ACT(out, in_, func, reads, writes, **kw):
        return em.op("act", lambda e: e.activation(out=out, in_=in_, func=func, **kw), reads, writes)

    def MM(out, lhsT, rhs, reads, writes, start=True, stop=True):
        return em.op("pe", lambda e: e.matmul(out, lhsT=lhsT, rhs=rhs, start=start, stop=stop), reads, writes)

    def TR(out, in_, ident, reads, writes):
        return em.op("pe", lambda e: e.transpose(out, in_, ident), reads, writes)

    def TS(out, in0, s1, s2, op0, op1, reads, writes, eng="dve"):
        if op1 is None:
            return em.op(eng, lambda e: e.tensor_scalar(out=out, in0=in0, scalar1=s1, scalar2=None, op0=op0), reads, writes)
        return em.op(eng, lambda e: e.tensor_scalar(out=out, in0=in0, scalar1=s1, scalar2=s2, op0=op0, op1=op1), reads, writes)

    def TT(out, in0, in1, op, reads, writes, eng="dve"):
        return em.op(eng, lambda e: e.tensor_tensor(out=out, in0=in0, in1=in1, op=op), reads, writes)

    def STT(out, in0, scalar, in1, op0, op1, reads, writes, eng="dve"):
        return em.op(eng, lambda e: e.scalar_tensor_tensor(out=out, in0=in0, scalar=scalar, in1=in1, op0=op0, op1=op1), reads, writes)

    def CP(out, in_, reads, writes, eng="dve"):
        if eng == "act":
            return ACT(out, in_, AF.Copy, reads, writes)
        return em.op(eng, lambda e: e.tensor_copy(out=out, in_=in_), reads, writes)

    def MEMSET(out, val, writes, eng="dve"):
        return em.op(eng, lambda e: e.memset(out, val), (), writes)

    def LD(eng, buf, out_ap, in_ap, rbufs=(), partial=False):
        f = em.op_partial_write if partial else em.op
        return f(eng, lambda e: e.dma_start(out=out_ap, in_=in_ap), list(rbufs), [buf], dma_key=buf.name + "_ld")

    def ST(eng, out_ap, dbufs, buf, in_ap):
        return em.op_partial_write(eng, lambda e: e.dma_start(out=out_ap, in_=in_ap), [buf], list(dbufs), dma_key=buf.name + "_st")

    xnT = dram("xnT", [D, L], BF16)
    raw_tok = dram("raw_tok", [L, NTOK], F32)
    rawT = dram("rawT", [NFEAT, L], F32)
    hg_v = [dram(f"hg_v{j}", [L, 128], BF16) for j in range(2)]
    hg_kh = [[dram(f"hg_kh{j}{d}", [L, 128], BF16) for d in range(2)] for j in range(2)]
    hg_khT = [[dram(f"hg_khT{j}{d}", [128, L], BF16) for d in range(2)] for j in range(2)]
    hg_qhT = [[dram(f"hg_qhT{j}{d}", [128, L], BF16) for d in range(2)] for j in range(2)]
    o_hg = [[dram(f"o_hg{j}{d}", [L, 128], F32) for d in range(2)] for j in range(2)]
    gd_qT = [dram(f"gd_qT{j}", [128, L], BF16) for j in range(2)]
    gd_kT = [dram(f"gd_kT{j}", [128, L], BF16) for j in range(2)]
    gd_k = [dram(f"gd_k{j}", [L, 128], BF16) for j in range(2)]
    gd_v = [dram(f"gd_v{j}", [L, 128], BF16) for j in range(2)]
    gd_u = [[dram(f"gd_u{j}{d}", [L, 128], F32) for d in range(2)] for j in range(2)]
    gd_wT = [[dram(f"gd_wT{j}{d}", [128, L], BF16) for d in range(2)] for j in range(2)]
    gd_qk = [[dram(f"gd_qk{j}{d}", [L, 128], BF16) for d in range(2)] for j in range(2)]
    gd_kd = [[dram(f"gd_kd{j}{d}", [L, 128], BF16) for d in range(2)] for j in range(2)]
    o_gd = [[dram(f"o_gd{j}{d}", [L, 128], F32) for d in range(2)] for j in range(2)]

    finals = []
    try:
      with ExitStack() as es0:
          cst = sbt(es0, "cst", [128, C_END], F32)
          idb = sbt(es0, "idb", [128, 128], BF16)
          onb = sbt(es0, "onb", [128, 128], BF16)
          nwa = sbt(es0, "nwa", [128, KC], F32)
          lbt = sbt(es0, "lbt", [128, 8, 128], F32)
          LB = sbt(es0, "LB", [128, 4, 128], F32)
          OML = sbt(es0, "OML", [128, 4, 128], F32)
          NW = sbt(es0, "NW", [128, 256], F32)
          gdpt = sbt(es0, "gdpt", [128, 8], F32)
          NEGA = sbt(es0, "NEGA", [128, 4], F32)
          CW = sbt(es0, "CW", [128, 6, 5], F32)
          GDEC = sbt(es0, "GDEC", [128, 4, NCH + 2], F32)
          GALL = sbt(es0, "GALL", [128, NT, 4], F32)
          BALL = sbt(es0, "BALL", [128, NT, 4], F32)
          NBALL = sbt(es0, "NBALL", [128, NT, 4], F32)
          EG = sbt(es0, "EG", [128, 4, NT], F32)
          GL = sbt(es0, "GL", [128, 4, NT], F32)

          LD("sp", cst, cst[:], consts_d[:, :])
          LD("pool", idb, idb[:], consts_d[:, C_ID:C_ID + 128])
          LD("pool", onb, onb[:], consts_d[:, C_ONES:C_ONES + 128])
          LD("sp", nwa, nwa[:], anw[:, :])
          LD("sp", lbt, lbt[:].rearrange("p a b -> p (a b)"), lbp.partition_broadcast(128))
          LD("sp", NW, NW[:], nws.partition_broadcast(128))
          LD("sp", gdpt, gdpt[:], gdp.partition_broadcast(128))
          LD("sp", CW, CW[:], convw.rearrange("a p t -> p a t"))
          TT(LB[:], lbt[:, 0:4, :], lbt[:, 4:8, :], ALU.subtract, [lbt], [LB])
          ACT(LB[:], LB[:], AF.Sigmoid, [LB], [LB])
          TS(OML[:], LB[:], -1.0, 1.0, ALU.mult, ALU.add, [LB], [OML])
          ACT(NEGA[:], gdpt[:, 0:4], AF.Exp, [gdpt], [NEGA])
          TS(NEGA[:], NEGA[:], -1.0, None, ALU.mult, None, [NEGA], [NEGA])
          MEMSET(GDEC[:], 1.0, [GDEC])
          ident = cst[:, C_ID:C_ID + 128]
          ones_f = cst[:, C_ONES:C_ONES + 128]

          with ExitStack() as es:
              XB = [sbt(es, f"XB{i}", [128, KC, 512], F32) for i in range(1)]
              XN = [sbt(es, f"XN{i}", [128, KC, 512], BF16) for i in range(2)]
              SQ = [sbt(es, f"SQ{i}", [128, 512], BF16) for i in range(3)]
              rst = [sbt(es, f"rst{i}", [128, 512], F32) for i in range(2)]
              pss = [pst(es, f"pss{i}", [128, 512], F32) for i in range(2)]
              xT3 = xT.rearrange("(kc p) t -> p kc t", p=128)
              xn3 = xnT.rearrange("(kc p) t -> p kc t", p=128)
              for b in range(NB):
                  xb, xn, ps, rs = XB[0], XN[b % 2], pss[b % 2], rst[b % 2]
                  ts_ = slice(b * 512, (b + 1) * 512)
                  for g in range(4):
                      LD("sp", xb, xb[:, g * 8:(g + 1) * 8, :], xT3[:, g * 8:(g + 1) * 8, ts_], partial=(g > 0))
                  for kc in range(KC):
                      sq = SQ[kc % 3]
                      ACT(sq[:], xb[:, kc, :], AF.Square, [xb], [sq])
                      MM(ps[:], onb[:], sq[:], [onb, sq], [ps], start=(kc == 0), stop=(kc == KC - 1))
                  ACT(rs[:], ps[:], AF.Sqrt, [ps], [rs], scale=1.0 / D, bias=EPS)
                  em.op("dve", lambda e, rs=rs: e.reciprocal(out=rs[:], in_=rs[:]), [rs], [rs])
                  for kc in range(KC):
                      f = em.op if kc == 0 else em.op_partial_write
                      f("dve", lambda e, xn=xn, xb=xb, kc=kc, rs=rs: e.scalar_tensor_tensor(
                          out=xn[:, kc, :], in0=xb[:, kc, :], scalar=nwa[:, kc:kc + 1], in1=rs[:],
                          op0=ALU.mult, op1=ALU.mult), [xb, nwa, rs], [xn])
                  for g in range(4):
                      ST("sp", xn3[:, g * 8:(g + 1) * 8, ts_], [R("xnT", b)], xn, xn[:, g * 8:(g + 1) * 8, :])
          em.barrier()
          if stop_after <= 1:
              raise _Stop()

          with ExitStack() as es:
              WB = sbt(es, "WB", [128, KC, NTOK], BF16)
              XN = [sbt(es, f"XNb{i}", [128, KC, 512], BF16) for i in range(2)]
              stg = [sbt(es, f"stg{i}", [128, NTOK], F32) for i in range(1)]
              pp = [pst(es, f"pp{i}", [128, 512], F32) for i in range(4)]
              w3 = w_tok.rearrange("(kc p) n -> p kc n", p=128)
              for g in range(8):
                  LD("pool", WB, WB[:, g * 4:(g + 1) * 4, :], w3[:, g * 4:(g + 1) * 4, :], partial=(g > 0))
              cgs = [(0, 512), (512, 1024), (1024, 1536), (1536, NTOK)]
              it = 0
              for b in range(NB):
                  xn = XN[b % 2]
                  ts_ = slice(b * 512, (b + 1) * 512)
                  for g in range(4):
                      LD("sp", xn, xn[:, g * 8:(g + 1) * 8, :], xn3[:, g * 8:(g + 1) * 8, ts_], rbufs=[R("xnT", b)], partial=(g > 0))
                  for t in range(4):
                      sg = stg[0]
                      for ci, (c0, c1) in enumerate(cgs):
                          ps = pp[it % 4]
                          it += 1
                          for kc in range(KC):
                              MM(ps[:, 0:c1 - c0], xn[:, kc, t * 128:(t + 1) * 128], WB[:, kc, c0:c1], [xn, WB], [ps],
                                 start=(kc == 0), stop=(kc == KC - 1))
                          f = em.op if ci == 0 else em.op_partial_write
                          if ci % 2 == 0:
                              f("act", lambda e, sg=sg, ps=ps, c0=c0, c1=c1: e.activation(out=sg[:, c0:c1], in_=ps[:, 0:c1 - c0], func=AF.Copy), [ps], [sg])
                          else:
                              f("dve", lambda e, sg=sg, ps=ps, c0=c0, c1=c1: e.tensor_copy(out=sg[:, c0:c1], in_=ps[:, 0:c1 - c0]), [ps], [sg])
                      r0 = b * 512 + t * 128
                      ST("sp", raw_tok[r0:r0 + 128, :], [R("raw_tok", b)], sg, sg[:])
          em.barrier()
          if stop_after <= 2:
              raise _Stop()

          with ExitStack() as es:
              WC = sbt(es, "WC", [128, KC, NFEAT], BF16)
              XN = [sbt(es, f"XNc{i}", [128, KC, 512], BF16) for i in range(2)]
              stg = [sbt(es, f"stgc{i}", [128, 512], F32) for i in range(3)]
              pp = [pst(es, f"ppc{i}", [128, 512], F32) for i in range(4)]
              w3 = w_feat.rearrange("(kc p) n -> p kc n", p=128)
              for g in range(8):
                  LD("pool", WC, WC[:, g * 4:(g + 1) * 4, :], w3[:, g * 4:(g + 1) * 4, :], partial=(g > 0))
              it = 0
              for b in range(NB):
                  xn = XN[b % 2]
                  ts_ = slice(b * 512, (b + 1) * 512)
                  for g in range(4):
                      LD("sp", xn, xn[:, g * 8:(g + 1) * 8, :], xn3[:, g * 8:(g + 1) * 8, ts_], rbufs=[R("xnT", b)], partial=(g > 0))
                  for m in range(6):
                      ps = pp[it % 4]
                      sg = stg[it % 3]
                      it += 1
                      for kc in range(KC):
                          MM(ps[:], WC[:, kc, m * 128:(m + 1) * 128], xn[:, kc, :], [WC, xn], [ps], start=(kc == 0), stop=(kc == KC - 1))
                      CP(sg[:], ps[:], [ps], [sg], eng=("act" if m % 2 else "dve"))
                      ST("sp", rawT[m * 128:(m + 1) * 128, ts_], [R("rawT", b)], sg, sg[:])
          em.barrier()
          if stop_after <= 3:
              raise _Stop()

          with ExitStack() as es:
              RT = [sbt(es, f"RT{i}", [128, NTOK], F32) for i in range(2)]
              qs = sbt(es, "qs", [128, 128], F32)
              sgm = sbt(es, "sgm", [128, 128], F32)
              ff = sbt(es, "ff", [128, 128], F32)
              lf = sbt(es, "lf", [128, 128], F32)
              kk = sbt(es, "kk", [128, 128], F32)
              eD = sbt(es, "eD", [128, 128], F32)
              enD = sbt(es, "enD", [128, 128], F32)
              kh = [sbt(es, f"kh{i}", [128, 128], BF16) for i in range(2)]
              qh = sbt(es, "qh", [128, 128], BF16)
              khT = [sbt(es, f"khT{i}", [128, 128], BF16) for i in range(2)]
              qhT = [sbt(es, f"qhT{i}", [128, 128], BF16) for i in range(2)]
              vb = [sbt(es, f"vb{i}", [128, 128], BF16) for i in range(2)]
              gt = sbt(es, "gt", [128, 4], F32)
              pD = pst(es, "pD", [128, 128], F32)
              pG = pst(es, "pG", [128, 2], F32)
              pT1 = pst(es, "pT1", [128, 128], BF16)
              pT2 = pst(es, "pT2", [128, 128], BF16)
              it = 0
              for t in range(NT):
                  rt = RT[t % 2]
                  b = t // 4
                  LD("sp", rt, rt[:], raw_tok[t * 128:(t + 1) * 128, :], rbufs=[R("raw_tok", b)])
                  tsl = slice(t * 128, (t + 1) * 128)
                  TT(gt[:], rt[:, 1280:1284], gdpt[:, 4:8], ALU.add, [rt, gdpt], [gt])
                  ACT(gt[:], gt[:], AF.Exp, [gt], [gt])
                  ACT(gt[:], gt[:], AF.Ln, [gt], [gt], bias=1.0)
                  em.op_partial_write("dve", lambda e, t=t: e.tensor_tensor(out=GALL[:, t, :], in0=gt[:], in1=NEGA[:], op=ALU.mult), [gt, NEGA], [GALL])
                  em.op_partial_write("act", lambda e, t=t, rt=rt: e.activation(out=BALL[:, t, :], in_=rt[:, 1284:1288], func=AF.Sigmoid), [rt], [BALL])
                  for j in range(2):
                      ACT(qs[:], rt[:, j * 128:(j + 1) * 128], AF.Silu, [rt], [qs])
                      v_ = vb[j]
                      CP(v_[:], rt[:, 768 + j * 128:768 + (j + 1) * 128], [rt], [v_])
                      ST("sp", hg_v[j][tsl, :], [R(f"hg_v{j}", b)], v_, v_[:])
                      for d in range(2):
                          zc = 256 + d * 256 + j * 128
                          dj = d * 2 + j
                          khb, khTb, qhTb = kh[it % 2], khT[it % 2], qhT[it % 2]
                          it += 1
                          ACT(sgm[:], rt[:, zc:zc + 128], AF.Sigmoid, [rt], [sgm])
                          TT(ff[:], sgm[:], OML[:, dj, :], ALU.mult, [sgm, OML], [ff])
                          TT(ff[:], ff[:], LB[:, dj, :], ALU.add, [ff, LB], [ff])
                          ACT(lf[:], ff[:], AF.Ln, [ff], [lf])
                          TS(kk[:], ff[:], -1.0, 1.0, ALU.mult, ALU.add, [ff], [kk])
                          mcol = C_HMF if d == 0 else C_HMB
                          MM(pD[:], cst[:, mcol:mcol + 128], lf[:], [cst, lf], [pD])
                          MM(pG[:], lf[:], cst[:, C_CIND:C_CIND + 2], [cst, lf], [pG])
                          em.op_partial_write("act", lambda e, dj=dj, t=t: e.activation(out=GDEC[:, dj, 2 * t:2 * t + 2], in_=pG[:], func=AF.Exp), [pG], [GDEC])
                          ACT(eD[:], pD[:], AF.Exp, [pD], [eD])
                          ACT(enD[:], pD[:], AF.Exp, [pD], [enD], scale=-1.0)
                          TT(khb[:], kk[:], eD[:], ALU.mult, [kk, eD], [khb])
                          STT(qh[:], qs[:], 128 ** -0.5, enD[:], ALU.mult, ALU.mult, [qs, enD], [qh])
                          TR(pT1[:], khb[:], idb[:], [khb, idb], [pT1])
                          TR(pT2[:], qh[:], idb[:], [qh, idb], [pT2])
                          CP(khTb[:], pT1[:], [pT1], [khTb], eng="act")
                          CP(qhTb[:], pT2[:], [pT2], [qhTb], eng="dve")
                          ST("sp", hg_kh[j][d][tsl, :], [R(f"hg_kh{j}{d}", b)], khb, khb[:])
                          ST("sp", hg_khT[j][d][:, tsl], [R(f"hg_khT{j}{d}", b)], khTb, khTb[:])
                          ST("sp", hg_qhT[j][d][:, tsl], [R(f"hg_qhT{j}{d}", b)], qhTb, qhTb[:])
              TS(NBALL[:], BALL[:], -1.0, None, ALU.mult, None, [BALL], [NBALL])
          em.barrier()
          if stop_after <= 4:
              raise _Stop()

          with ExitStack() as es:
              XW = [sbt(es, f"XW{i}", [128, 516], F32) for i in range(2)]
              acc = sbt(es, "acc", [128, 512], F32)
              sl = sbt(es, "sl", [128, 512], F32)
              sq = sbt(es, "sq5", [128, 512], BF16)
              rn = sbt(es, "rn", [128, 512], F32)
              ob = [sbt(es, f"ob{i}", [128, 512], BF16) for i in range(2)]
              tk = [sbt(es, f"tk{i}", [128, 4, 128], BF16) for i in range(2)]
              pS5 = pst(es, "pS5", [128, 512], F32)
              pT5 = pst(es, "pT5", [128, 4, 128], BF16)
              it = 0
              for b in range(NB):
                  for j in range(2):
                      for w in range(3):
                          m = j * 3 + w
                          row0 = w * 256 + j * 128
                          xw = XW[it % 2]
                          obb, tkb = ob[it % 2], tk[it % 2]
                          it += 1
                          lo = b * 512 - 2
                          hi = b * 512 + 514
                          c_lo = 0
                          c_hi = 516
                          if b == 0:
                              lo, c_lo = 0, 2
                          if b == NB - 1:
                              hi, c_hi = L, 514
                          rb = [R("rawT", bb) for bb in (b - 1, b, b + 1) if 0 <= bb < NB]
                          MEMSET(xw[:], 0.0, [xw])
                          LD("sp", xw, xw[:, c_lo:c_hi], rawT[row0:row0 + 128, lo:hi], rbufs=rb, partial=True)
                          TS(acc[:], xw[:, 0:512], CW[:, m, 0:1], None, ALU.mult, None, [xw, CW], [acc])
                          for tp in range(1, 5):
                              STT(acc[:], xw[:, tp:tp + 512], CW[:, m, tp:tp + 1], acc[:], ALU.mult, ALU.add, [xw, CW, acc], [acc])
                          ACT(sl[:], acc[:], AF.Silu, [acc], [sl])
                          ts_ = slice(b * 512, (b + 1) * 512)
                          if w < 2:
                              ACT(sq[:], sl[:], AF.Square, [sl], [sq])
                              MM(pS5[:], onb[:], sq[:], [onb, sq], [pS5])
                              ACT(rn[:], pS5[:], AF.Sqrt, [pS5], [rn], bias=EPS)
                              em.op("dve", lambda e: e.reciprocal(out=rn[:], in_=rn[:]), [rn], [rn])
                              sc = (128 ** -0.5) if w == 0 else 1.0
                              STT(obb[:], sl[:], sc, rn[:], ALU.mult, ALU.mult, [sl, rn], [obb])
                              dst = gd_qT[j] if w == 0 else gd_kT[j]
                              nm = f"gd_qT{j}" if w == 0 else f"gd_kT{j}"
                              ST("sp", dst[:, ts_], [R(nm, b)], obb, obb[:])
                          else:
                              CP(obb[:], sl[:], [sl], [obb])
                          if w >= 1:
                              for q4 in range(4):
                                  f = em.op if q4 == 0 else em.op_partial_write
                                  f("pe", lambda e, q4=q4, obb=obb: e.transpose(pT5[:, q4, :], obb[:, q4 * 128:(q4 + 1) * 128], idb[:]), [obb, idb], [pT5])
                              CP(tkb[:], pT5[:], [pT5], [tkb], eng="act")
                              dst = gd_k[j] if w == 1 else gd_v[j]
                              nm = f"gd_k{j}" if w == 1 else f"gd_v{j}"
                              ST("sp", dst[ts_, :].rearrange("(q p) n -> p q n", p=128), [R(nm, b)], tkb, tkb[:])
          em.barrier()
          if stop_after <= 5:
              raise _Stop()

          with ExitStack() as es:
              kTb = [sbt(es, f"kTb{i}", [128, 512], BF16) for i in range(2)]
              qTb = [sbt(es, f"qTb{i}", [128, 512], BF16) for i in range(2)]
              kt4 = [sbt(es, f"kt4{i}", [128, 4, 128], BF16) for i in range(2)]
              vt4 = [sbt(es, f"vt4{i}", [128, 4, 128], BF16) for i in range(2)]
              gc = sbt(es, "gc", [128, 2], F32)
              sc4 = sbt(es, "sc4", [128, 4], F32)
              dg = sbt(es, "dg", [128, 128], F32)
              t1 = sbt(es, "t1", [128, 128], F32)
              t2 = sbt(es, "t2", [128, 128], F32)
              rels = sbt(es, "rels", [128, 128], F32)
              relT = sbt(es, "relT", [128, 128], F32)
              P = [sbt(es, f"P{i}", [128, 128], F32) for i in range(2)]
              PT = [sbt(es, f"PT{i}", [128, 128], F32) for i in range(2)]
              X = [sbt(es, f"X{i}", [128, 256], F32) for i in range(2)]
              wbf = sbt(es, "wbf", [128, 128], BF16)
              wTs = [sbt(es, f"wTs{i}", [128, 128], BF16) for i in range(2)]
              qks = [sbt(es, f"qks{i}", [128, 128], BF16) for i in range(2)]
              kds = [sbt(es, f"kds{i}", [128, 128], BF16) for i in range(2)]
              us = [sbt(es, f"us{i}", [128, 128], F32) for i in range(2)]
              pg = pst(es, "pg6", [128, 2], F32)
              pR = pst(es, "pR", [128, 128], F32)
              pK = pst(es, "pK", [128, 128], F32)
              pQ = pst(es, "pQ", [128, 128], F32)
              pX = pst(es, "pX", [128, 256], F32)
              pP = pst(es, "pP", [128, 128], F32)
              pPT = pst(es, "pPT", [128, 128], F32)
              pW = pst(es, "pW", [128, 128], BF16)
              it = 0
              for b in range(NB):
                  for j in range(2):
                      kT_, qT_, k4, v4 = kTb[(b * 2 + j) % 2], qTb[(b * 2 + j) % 2], kt4[(b * 2 + j) % 2], vt4[(b * 2 + j) % 2]
                      ts_ = slice(b * 512, (b + 1) * 512)
                      LD("sp", kT_, kT_[:], gd_kT[j][:, ts_], rbufs=[R(f"gd_kT{j}", b)])
                      LD("sp", qT_, qT_[:], gd_qT[j][:, ts_], rbufs=[R(f"gd_qT{j}", b)])
                      LD("sp", k4, k4[:], gd_k[j][ts_, :].rearrange("(q p) n -> p q n", p=128), rbufs=[R(f"gd_k{j}", b)])
                      LD("sp", v4, v4[:], gd_v[j][ts_, :].rearrange("(q p) n -> p q n", p=128), rbufs=[R(f"gd_v{j}", b)])
                      for q4 in range(4):
                          t = b * 4 + q4
                          tsl = slice(t * 128, (t + 1) * 128)
                          cs = slice(q4 * 128, (q4 + 1) * 128)
                          for d in range(2):
                              dj = d * 2 + j
                              i2 = it % 2
                              it += 1
                              ucol = C_UF if d == 0 else C_UB
                              mbs = (C_MBS if d == 0 else C_MBST)
                              mbiT = (C_MBIT if d == 0 else C_MBI)
                              gcol = GALL[:, t, dj:dj + 1]
                              MM(pg[:, 0:1], cst[:, ucol:ucol + 128], gcol, [cst, GALL], [pg])
                              em.op_partial_write("pe", lambda e, gcol=gcol: e.matmul(pg[:, 1:2], lhsT=ones_f, rhs=gcol, start=True, stop=True), [cst, GALL], [pg])
                              CP(gc[:], pg[:], [pg], [gc])
                              em.op_partial_write("act", lambda e, dj=dj, t=t: e.activation(out=EG[:, dj, t:t + 1], in_=gc[:, 0:1], func=AF.Exp), [gc], [EG])
                              em.op_partial_write("act", lambda e, dj=dj, t=t: e.activation(out=GL[:, dj, t:t + 1], in_=gc[:, 1:2], func=AF.Exp), [gc], [GL])
                              ACT(sc4[:, 0:1], gc[:, 0:1], AF.Exp, [gc], [sc4], scale=-1.0, bias=gc[:, 1:2])
                              em.op_partial_write("dve", lambda e, dj=dj, t=t: e.tensor_tensor(out=sc4[:, 1:2], in0=EG[:, dj, t:t + 1], in1=BALL[:, t, dj:dj + 1], op=ALU.mult), [EG, BALL], [sc4])
                              TS(dg[:], ident, gc[:, 0:1], None, ALU.mult, None, [cst, gc], [dg])
                              MM(pR[:], ones_f, dg[:], [cst, dg], [pR])
                              TS(t1[:], pR[:], gc[:, 0:1], -1.0, ALU.subtract, ALU.mult, [pR, gc], [t1])
                              TT(t1[:], t1[:], cst[:, mbs:mbs + 128], ALU.add, [t1, cst], [t1])
                              ACT(rels[:], t1[:], AF.Exp, [t1], [rels])
                              TS(t2[:], pR[:], gc[:, 0:1], None, ALU.subtract, None, [pR, gc], [t2])
                              TT(t2[:], t2[:], cst[:, mbiT:mbiT + 128], ALU.add, [t2, cst], [t2])
                              ACT(relT[:], t2[:], AF.Exp, [t2], [relT])
                              MM(pK[:], kT_[:, cs], kT_[:, cs], [kT_], [pK])
                              p_, pt_ = P[0], PT[0]
                              STT(p_[:], pK[:], NBALL[:, t, dj:dj + 1], rels[:], ALU.mult, ALU.mult, [pK, NBALL, rels], [p_])
                              TR(pPT[:], p_[:], ident, [p_, cst], [pPT])
                              CP(pt_[:], pPT[:], [pPT], [pt_], eng="act")
                              MM(pQ[:], kT_[:, cs], qT_[:, cs], [kT_, qT_], [pQ])
                              qk_ = qks[i2]
                              TT(qk_[:], pQ[:], relT[:], ALU.mult, [pQ, relT], [qk_])
                              ST("sp", gd_qk[j][d][tsl, :], [R(f"gd_qk{j}{d}", b)], qk_, qk_[:])
                              kd_ = kds[i2]
                              TS(kd_[:], k4[:, q4, :], sc4[:, 0:1], None, ALU.mult, None, [k4, sc4], [kd_], eng="pool")
                              ST("sp", gd_kd[j][d][tsl, :], [R(f"gd_kd{j}{d}", b)], kd_, kd_[:])
                              x_ = X[0]
                              TS(x_[:, 0:128], v4[:, q4, :], BALL[:, t, dj:dj + 1], None, ALU.mult, None, [v4, BALL], [x_], eng="pool")
                              em.op_partial_write("pool", lambda e, x_=x_, k4=k4, q4=q4: e.tensor_scalar(out=x_[:, 128:256], in0=k4[:, q4, :], scalar1=sc4[:, 1:2], scalar2=None, op0=ALU.mult), [k4, sc4], [x_])
                              cur = 0
                              for m in range(7):
                                  xs, xd = X[cur], X[1 - cur]
                                  pc, ptc = P[cur], PT[cur]
                                  MM(pX[:], ptc[:], xs[:], [ptc, xs], [pX])
                                  TT(xd[:], xs[:], pX[:], ALU.add, [xs, pX], [xd])
                                  if m < 6:
                                      pn, ptn = P[1 - cur], PT[1 - cur]
                                      MM(pP[:], ptc[:], pc[:], [ptc, pc], [pP])
                                      MM(pPT[:], pc[:], ptc[:], [ptc, pc], [pPT])
                                      CP(pn[:], pP[:], [pP], [pn], eng="act")
                                      CP(ptn[:], pPT[:], [pPT], [ptn], eng="act")
                                  cur = 1 - cur
                              xf = X[cur]
                              u_ = us[i2]
                              CP(u_[:], xf[:, 0:128], [xf], [u_], eng="pool")
                              ST("sp", gd_u[j][d][tsl, :], [R(f"gd_u{j}{d}", b)], u_, u_[:])
                              CP(wbf[:], xf[:, 128:256], [xf], [wbf])
                              TR(pW[:], wbf[:], idb[:], [wbf, idb], [pW])
                              wT_ = wTs[i2]
                              CP(wT_[:], pW[:], [pW], [wT_])
                              ST("sp", gd_wT[j][d][:, tsl], [R(f"gd_wT{j}{d}", b)], wT_, wT_[:])
          em.barrier()
          if stop_after <= 6:
              raise _Stop()

          with ExitStack() as es:
              hS = [sbt(es, f"hS{i}", [128, 128], F32) for i in range(4)]
              hSp = [sbt(es, f"hSp{i}", [128, 128], BF16) for i in range(4)]
              hkT = [sbt(es, f"hkT{i}", [128, 512], BF16) for i in range(4)]
              hqT = [sbt(es, f"hqT{i}", [128, 512], BF16) for i in range(4)]
              hk = [sbt(es, f"hk{i}", [64, 8, 128], BF16) for i in range(4)]
              hv = [sbt(es, f"hv{i}", [64, 8, 128], BF16) for i in range(4)]
              hos = [sbt(es, f"hos{i}", [64, 8, 128], F32) for i in range(4)]
              ham = [sbt(es, f"ham{i}", [64, 64], BF16) for i in range(4)]
              def split4(name, w):
                  t_ = pst(es, name, [128, 4, w], F32)
                  return [Buf(t_.ap[:, i, :], f"{name}{i}") for i in range(4)]
              pha = split4("pha", 64)
              pho = split4("pho", 128)
              phs = split4("phs", 128)
              gS = [sbt(es, f"gS{i}", [128, 128], F32) for i in range(4)]
              gSb = [sbt(es, f"gSb{i}", [128, 128], BF16) for i in range(4)]
              gwT = [sbt(es, f"gwT{i}", [128, 512], BF16) for i in range(4)]
              gqT = [sbt(es, f"gqT{i}", [128, 512], BF16) for i in range(4)]
              gu = [sbt(es, f"gu{i}", [128, 4, 128], F32) for i in range(4)]
              gqk = [sbt(es, f"gqk{i}", [128, 4, 128], BF16) for i in range(4)]
              gkd = [sbt(es, f"gkd{i}", [128, 4, 128], BF16) for i in range(4)]
              gos = [sbt(es, f"gos{i}", [128, 4, 128], F32) for i in range(4)]
              gvn = [sbt(es, f"gvn{i}", [128, 128], BF16) for i in range(4)]
              gob = [sbt(es, f"gob{i}", [128, 128], F32) for i in range(4)]
              pgA = split4("pgA", 128)
              pgB = split4("pgB", 128)
              pgC = split4("pgC", 128)
              pgS = split4("pgS", 128)
              for i in range(4):
                  MEMSET(hS[i][:], 0.0, [hS[i]])
                  MEMSET(hSp[i][:], 0.0, [hSp[i]])
                  MEMSET(gS[i][:], 0.0, [gS[i]])
                  MEMSET(gSb[i][:], 0.0, [gSb[i]])
              for bi in range(NB):
                  for j in range(2):
                      for d in range(2):
                          dj = d * 2 + j
                          b = bi if d == 0 else NB - 1 - bi
                          ts_ = slice(b * 512, (b + 1) * 512)
                          LD("sp", hkT[dj], hkT[dj][:], hg_khT[j][d][:, ts_], rbufs=[R(f"hg_khT{j}{d}", b)])
                          LD("sp", hqT[dj], hqT[dj][:], hg_qhT[j][d][:, ts_], rbufs=[R(f"hg_qhT{j}{d}", b)])
                          LD("sp", hk[dj], hk[dj][:], hg_kh[j][d][ts_, :].rearrange("(c p) n -> p c n", p=64), rbufs=[R(f"hg_kh{j}{d}", b)])
                          LD("sp", hv[dj], hv[dj][:], hg_v[j][ts_, :].rearrange("(c p) n -> p c n", p=64), rbufs=[R(f"hg_v{j}", b)])
                          LD("sp", gwT[dj], gwT[dj][:], gd_wT[j][d][:, ts_], rbufs=[R(f"gd_wT{j}{d}", b)])
                          LD("sp", gqT[dj], gqT[dj][:], gd_qT[j][:, ts_], rbufs=[R(f"gd_qT{j}", b)])
                          LD("sp", gu[dj], gu[dj][:], gd_u[j][d][ts_, :].rearrange("(q p) n -> p q n", p=128), rbufs=[R(f"gd_u{j}{d}", b)])
                          LD("sp", gqk[dj], gqk[dj][:], gd_qk[j][d][ts_, :].rearrange("(q p) n -> p q n", p=128), rbufs=[R(f"gd_qk{j}{d}", b)])
                          LD("sp", gkd[dj], gkd[dj][:], gd_kd[j][d][ts_, :].rearrange("(q p) n -> p q n", p=128), rbufs=[R(f"gd_kd{j}{d}", b)])
                  for step in range(8):
                      for dj in range(4):
                          d, j = dj // 2, dj % 2
                          b = bi if d == 0 else NB - 1 - bi
                          c = step if d == 0 else 7 - step
                          gch = b * 8 + c
                          nxt = gch + (1 if d == 0 else -1)
                          nxt_col = nxt if 0 <= nxt < NCH else NCH
                          cs = slice(c * 64, (c + 1) * 64)
                          kcol = C_HKF if d == 0 else C_HKB
                          em.op("pe", lambda e, dj=dj, cs=cs: e.matmul(pha[dj][0:64, :], lhsT=hkT[dj][:, cs], rhs=hqT[dj][:, cs], start=True, stop=True), [hkT[dj], hqT[dj]], [pha[dj]])
                          TT(ham[dj][:], pha[dj][0:64, :], cst[0:64, kcol:kcol + 64], ALU.mult, [pha[dj], cst], [ham[dj]])
                          em.op("pe", lambda e, dj=dj, c=c: e.matmul(pho[dj][0:64, :], lhsT=ham[dj][:], rhs=hv[dj][:, c, :], start=True, stop=False), [ham[dj], hv[dj]], [pho[dj]])
                          em.op_partial_write("pe", lambda e, dj=dj, cs=cs: e.matmul(pho[dj][0:64, :], lhsT=hqT[dj][:, cs], rhs=hSp[dj][:], start=False, stop=True), [hqT[dj], hSp[dj]], [pho[dj]])
                          em.op_partial_write("act", lambda e, dj=dj, c=c: e.activation(out=hos[dj][:, c, :], in_=pho[dj][0:64, :], func=AF.Copy), [pho[dj]], [hos[dj]])
                          em.op("pe", lambda e, dj=dj, c=c: e.matmul(phs[dj][:], lhsT=hk[dj][:, c, :], rhs=hv[dj][:, c, :], start=True, stop=True), [hk[dj], hv[dj]], [phs[dj]])
                          STT(hS[dj][:], hS[dj][:], GDEC[:, dj, gch:gch + 1], phs[dj][:], ALU.mult, ALU.add, [hS[dj], GDEC, phs[dj]], [hS[dj]])
                          TS(hSp[dj][:], hS[dj][:], GDEC[:, dj, nxt_col:nxt_col + 1], None, ALU.mult, None, [hS[dj], GDEC], [hSp[dj]], eng="dve")
                      if step % 2 == 1 and os.environ.get('NOGD') is None:
                          for dj in range(4):
                              d, j = dj // 2, dj % 2
                              b = bi if d == 0 else NB - 1 - bi
                              q4 = (step // 2) if d == 0 else 3 - (step // 2)
                              t = b * 4 + q4
                              cs = slice(q4 * 128, (q4 + 1) * 128)
                              _k = int(os.environ.get('GDK', '99'))
                              if _k > 0:
                                  em.op("pe", lambda e, dj=dj, cs=cs: e.matmul(pgA[dj][:], lhsT=gwT[dj][:, cs], rhs=gSb[dj][:], start=True, stop=True), [gwT[dj], gSb[dj]], [pgA[dj]])
                              if _k > 1:
                                  em.op("pe", lambda e, dj=dj, cs=cs: e.matmul(pgB[dj][:], lhsT=gqT[dj][:, cs], rhs=gSb[dj][:], start=True, stop=True), [gqT[dj], gSb[dj]], [pgB[dj]])
                              if _k > 2:
                                  TT(gvn[dj][:], gu[dj][:, q4, :], pgA[dj][:], ALU.subtract, [gu[dj], pgA[dj]], [gvn[dj]])
                              if _k > 3:
                                  em.op("pe", lambda e, dj=dj, q4=q4: e.matmul(pgC[dj][:], lhsT=gqk[dj][:, q4, :], rhs=gvn[dj][:], start=True, stop=True), [gqk[dj], gvn[dj]], [pgC[dj]])
                              if _k > 4:
                                  ACT(gob[dj][:], pgB[dj][:], AF.Copy, [pgB[dj], EG], [gob[dj]], scale=EG[:, dj, t:t + 1])
                              if _k > 5:
                                  em.op_partial_write("dve", lambda e, dj=dj, q4=q4: e.tensor_tensor(out=gos[dj][:, q4, :], in0=gob[dj][:], in1=pgC[dj][:], op=ALU.add), [gob[dj], pgC[dj]], [gos[dj]])
                              if _k > 6:
                                  em.op("pe", lambda e, dj=dj, q4=q4: e.matmul(pgS[dj][:], lhsT=gkd[dj][:, q4, :], rhs=gvn[dj][:], start=True, stop=True), [gkd[dj], gvn[dj]], [pgS[dj]])
                              if _k > 7:
                                  STT(gS[dj][:], gS[dj][:], GL[:, dj, t:t + 1], pgS[dj][:], ALU.mult, ALU.add, [gS[dj], GL, pgS[dj]], [gS[dj]])
                              if _k > 8:
                                  CP(gSb[dj][:], gS[dj][:], [gS[dj]], [gSb[dj]], eng="dve")
                  for j in range(2):
                      for d in range(2):
                          dj = d * 2 + j
                          b = bi if d == 0 else NB - 1 - bi
                          ts_ = slice(b * 512, (b + 1) * 512)
                          ST("sp", o_hg[j][d][ts_, :].rearrange("(c p) n -> p c n", p=64), [R(f"o_hg{j}{d}", b)], hos[dj], hos[dj][:])
                          ST("sp", o_gd[j][d][ts_, :].rearrange("(q p) n -> p q n", p=128), [R(f"o_gd{j}{d}", b)], gos[dj], gos[dj][:])
          em.barrier()
          if stop_after <= 7:
              raise _Stop()

          with ExitStack() as es:
              RT = [sbt(es, f"RT8{i}", [128, NTOK], F32) for i in range(2)]
              oa = [sbt(es, f"oa{i}", [128, 128], F32) for i in range(2)]
              ob_ = [sbt(es, f"ob8{i}", [128, 128], F32) for i in range(2)]
              osum = sbt(es, "osum", [128, 128], F32)
              junk = sbt(es, "junk8", [128, 128], F32)
              ssq = sbt(es, "ssq", [128, 2], F32)
              sz = sbt(es, "sz", [128, 128], F32)
              yy = sbt(es, "yy", [128, 128], F32)
              yT = [sbt(es, f"yT{i}", [128, 128], F32) for i in range(2)]
              pY = pst(es, "pY", [128, 128], F32)
              it = 0
              for t in range(NT):
                  rt = RT[t % 2]
                  b = t // 4
                  tsl = slice(t * 128, (t + 1) * 128)
                  LD("sp", rt, rt[:], raw_tok[tsl, :], rbufs=[R("raw_tok", b)])
                  for g in range(2):
                      for j in range(2):
                          a_, b_ = oa[it % 2], ob_[it % 2]
                          yT_ = yT[it % 2]
                          it += 1
                          src = o_hg if g == 0 else o_gd
                          nm = "o_hg" if g == 0 else "o_gd"
                          LD("sp", a_, a_[:], src[j][0][tsl, :], rbufs=[R(f"{nm}{j}0", b)])
                          LD("sp", b_, b_[:], src[j][1][tsl, :], rbufs=[R(f"{nm}{j}1", b)])
                          TT(osum[:], a_[:], b_[:], ALU.add, [a_, b_], [osum])
                          ACT(junk[:], osum[:], AF.Square, [osum], [junk, ssq], accum_out=ssq[:, 0:1])
                          ACT(ssq[:, 1:2], ssq[:, 0:1], AF.Sqrt, [ssq], [ssq], scale=1.0 / 128, bias=EPS)
                          em.op("dve", lambda e: e.reciprocal(out=ssq[:, 1:2], in_=ssq[:, 1:2]), [ssq], [ssq])
                          zc = (1024 if g == 0 else 1288) + j * 128
                          ACT(sz[:], rt[:, zc:zc + 128], AF.Silu, [rt], [sz])
                          STT(yy[:], osum[:], ssq[:, 1:2], NW[:, g * 128:(g + 1) * 128], ALU.mult, ALU.mult, [osum, ssq, NW], [yy])
                          TT(yy[:], yy[:], sz[:], ALU.mult, [yy, sz], [yy])
                          TR(pY[:], yy[:], ident, [yy, cst], [pY])
                          CP(yT_[:], pY[:], [pY], [yT_], eng="act")
                          slot = g * 2 + j
                          finals.append(em.op("sp", lambda e, slot=slot, tsl=tsl, yT_=yT_: e.dma_start(out=outT[slot * 128:(slot + 1) * 128, tsl], in_=yT_[:]),
                                              [yT_], [], dma_key=yT_.name + "_st"))
    except _Stop:
        pass
    lastk = {}
    for oid in finals:
        lastk[em.ops[oid].dma_sem] = oid
    if not finals:
        for o in em.ops:
            if o.is_dma:
                lastk[o.dma_sem] = o.id
    em.emit(final_wait_ops=list(lastk.values()))
    print("P1 ops:", em.counts, "sems:", em.n_sems)
    return nc


def prep_p1(inp, L, r):
    f32 = np.float32
    x = np.asarray(inp["x"], f32)[0]
    w_in = np.asarray(inp["w_in"], f32)[0]
    HGW = 2048
    h = [2 * r, 2 * r + 1]
    def hc(base, hh):
        return np.arange(base + hh * 128, base + (hh + 1) * 128)
    cols = []
    for base in (0, HGW, 2 * HGW, 3 * HGW, 4 * HGW):
        for hh in h:
            cols.append(hc(base, hh))
    g0 = 5 * HGW
    a0 = g0 + 6144
    b0 = a0 + 32
    z0 = b0 + 32
    ab = [a0 + d * 16 + hh for d in range(2) for hh in h] + [b0 + d * 16 + hh for d in range(2) for hh in h]
    cols.append(np.array(ab))
    for hh in h:
        cols.append(hc(z0, hh))
    tokc = np.concatenate(cols)
    assert tokc.size == NTOK
    featc = np.concatenate([hc(g0 + w * 2048, hh) for w in range(3) for hh in h])
    lb = np.asarray(inp["hg_lower_bound"], f32)
    lbp = np.stack([np.stack([np.stack([lb[l, d, hh * 128:(hh + 1) * 128] for hh in h]) for d in range(2)]) for l in range(2)])
    cw = np.asarray(inp["gd_conv_w"], f32)[0]
    convw = np.stack([cw[:, w * 2048 + hh * 128: w * 2048 + (hh + 1) * 128].T for hh in h for w in range(3)])
    al = np.asarray(inp["gd_A_log"], f32)[0]
    db = np.asarray(inp["gd_dt_bias"], f32)[0]
    gdp = np.array([al[d, hh] for d in range(2) for hh in h] + [db[d, hh] for d in range(2) for hh in h], f32)[None, :]
    nws = np.concatenate([np.asarray(inp["hg_out_norm_w"], f32)[0], np.asarray(inp["gd_out_norm_w"], f32)[0]])[None, :]
    return dict(
        xT=np.ascontiguousarray(x.T),
        w_tok=np.ascontiguousarray(w_in[:, tokc]),
        w_feat=np.ascontiguousarray(w_in[:, featc]),
        consts=make_consts(),
        anw=np.ascontiguousarray(np.asarray(inp["attn_norm_w"], f32)[0].reshape(KC, 128).T),
        lbp=np.ascontiguousarray(lbp.reshape(1, -1)),
        convw=np.ascontiguousarray(convw),
        gdp=gdp, nws=nws,
    )


import numpy as np, os
from contextlib import ExitStack
import concourse.bass as bass
import concourse.mybir as mybir

D = 4096
KC = 32
NE = 16384
EPS = 1e-6


def build_p2(TC):
    nc = bass.Bass("TRN2", target_bir_lowering=False)
    NTL = TC // 128
    dt_in = lambda n, s: nc.dram_tensor(n, s, F32, kind="ExternalInput").ap()
    x_tok = dt_in("x_tok", [TC, D])
    p_tok = dt_in("p_tok", [TC, 256])
    mixT = dt_in("mixT", [D, TC])
    w_out = dt_in("w_out", [D, D])
    w_q = dt_in("w_q", [D, 2048])
    keysT = dt_in("keysT", [128, 16, 128])
    UT = dt_in("UT", [D, NE])
    V = dt_in("V", [NE, D])
    w_gate = dt_in("w_gate", [D, D])
    w_proj = dt_in("w_proj", [256, D])
    nws = dt_in("nws", [4, D])
    ident_d = dt_in("ident", [128, 128])
    y = nc.dram_tensor("y", [TC, D], F32, kind="ExternalOutput").ap()

    em = Emitter(nc)

    def sbt(es, name, shape, dt):
        h = es.enter_context(nc.sbuf_tensor(name, shape, dt))
        return Buf(h.ap() if hasattr(h, "ap") else h, name)

    def pst(es, name, shape, dt):
        h = es.enter_context(nc.psum_tensor(name, shape, dt))
        return Buf(h.ap() if hasattr(h, "ap") else h, name)

    def ACT(out, in_, func, reads, writes, **kw):
        return em.op("act", lambda e: e.activation(out=out, in_=in_, func=func, **kw), reads, writes)

    def ACTp(out, in_, func, reads, writes, **kw):
        return em.op_partial_write("act", lambda e: e.activation(out=out, in_=in_, func=func, **kw), reads, writes)

    def MM(out, lhsT, rhs, reads, writes, start=True, stop=True):
        return em.op("pe", lambda e: e.matmul(out, lhsT=lhsT, rhs=rhs, start=start, stop=stop), reads, writes)

    def MMp(out, lhsT, rhs, reads, writes, start=True, stop=True):
        return em.op_partial_write("pe", lambda e: e.matmul(out, lhsT=lhsT, rhs=rhs, start=start, stop=stop), reads, writes)

    def TS(out, in0, s1, s2, op0, op1, reads, writes, eng="dve", partial=False):
        f = em.op_partial_write if partial else em.op
        if op1 is None:
            return f(eng, lambda e: e.tensor_scalar(out=out, in0=in0, scalar1=s1, scalar2=None, op0=op0), reads, writes)
        return f(eng, lambda e: e.tensor_scalar(out=out, in0=in0, scalar1=s1, scalar2=s2, op0=op0, op1=op1), reads, writes)

    def TT(out, in0, in1, op, reads, writes, eng="dve", partial=False):
        f = em.op_partial_write if partial else em.op
        return f(eng, lambda e: e.tensor_tensor(out=out, in0=in0, in1=in1, op=op), reads, writes)

    def STT(out, in0, scalar, in1, op0, op1, reads, writes, eng="dve", partial=False):
        f = em.op_partial_write if partial else em.op
        return f(eng, lambda e: e.scalar_tensor_tensor(out=out, in0=in0, scalar=scalar, in1=in1, op0=op0, op1=op1), reads, writes)

    def CP(out, in_, reads, writes, eng="dve", partial=False):
        f = em.op_partial_write if partial else em.op
        if eng == "act":
            return f("act", lambda e: e.activation(out=out, in_=in_, func=AF.Copy), reads, writes)
        return f(eng, lambda e: e.tensor_copy(out=out, in_=in_), reads, writes)

    def LD(eng, buf, out_ap, in_ap, partial=False):
        f = em.op_partial_write if partial else em.op
        return f(eng, lambda e: e.dma_start(out=out_ap, in_=in_ap), [], [buf], dma_key=buf.name + "_ld")

    finals = []
    with ExitStack() as es:
        idb = sbt(es, "idb", [128, 128], BF16)
        kT = sbt(es, "kT", [128, 16, 128], BF16)
        NWB = sbt(es, "NWB", [128, D], F32)
        xt = sbt(es, "xt", [128, D], F32)
        h = sbt(es, "h", [128, D], F32)
        WS = [sbt(es, "WS0", [128, KC, 512], BF16)]
        xn = sbt(es, "xn", [128, D], BF16)
        junk = xn
        xnT = sbt(es, "xnT", [128, KC, 128], BF16)
        mT = xnT
        st2 = sbt(es, "st2", [128, 4], F32)
        WQ = [sbt(es, f"WQ{i}", [128, KC, 128], BF16) for i in range(1)]
        qT = sbt(es, "qT", [128, 16, 128], BF16)
        S = sbt(es, "S", [128, 16, 128], F32)
        wk = sbt(es, "wk", [128, 256], F32)
        t1 = sbt(es, "t1", [128, 16], F32)
        t2 = sbt(es, "t2", [128, 16], F32)
        cand = sbt(es, "cand", [128, 16, 16], F32)
        ct = sbt(es, "ct", [128, 16], F32)
        ez = sbt(es, "ez", [128, 16], F32)
        sc = sbt(es, "sc", [128, 8], F32)
        E1Z = sbt(es, "E1Z", [128, 8, 128], F32)
        E2 = sbt(es, "E2", [128, 8, 128], F32)
        TH = sbt(es, "TH", [128, 8, 128], F32)
        gm = [sbt(es, f"gm{i}", [128, 128], F32) for i in range(2)]
        gh = [sbt(es, f"gh{i}", [128, 128], BF16) for i in range(2)]
        UTt = [sbt(es, f"UTt{i}", [128, KC, 128], BF16) for i in range(2)]
        gel = [sbt(es, f"gel{i}", [128, 128], F32) for i in range(2)]
        WT = sbt(es, "WT", [128, 128, 128], BF16)
        pt = sbt(es, "pt", [128, 256], F32)
        ptb = sbt(es, "ptb", [128, 256], BF16)
        pTT = sbt(es, "pTT", [128, 2, 128], BF16)
        WP = sbt(es, "WP", [128, 2, D], BF16)
        emb = xt
        gate = sbt(es, "gate", [128, 512], F32)
        pA = [pst(es, f"pA{i}", [128, 512], F32) for i in range(2)]
        pTr = pst(es, "pTr", [128, 4, 128], BF16)
        pQ = [pst(es, "pQ0", [128, 128], F32)] * 2
        pG = [pst(es, f"pG{i}", [128, 128], F32) for i in range(2)]
        pH = [pst(es, f"pH{i}", [128, 128], F32) for i in range(2)]

        LD("pool", idb, idb[:], ident_d[:, :])
        LD("pool", kT, kT[:], keysT[:, :, :])
        LD("pool", WP, WP[:], w_proj.rearrange("(c p) n -> p c n", p=128))
        wo3 = w_out.rearrange("(kc p) n -> p kc n", p=128)
        wq3 = w_q.rearrange("(kc p) n -> p kc n", p=128)
        wg3 = w_gate.rearrange("(kc p) n -> p kc n", p=128)
        ut3 = UT.rearrange("(kc p) n -> p kc n", p=128)
        v3 = V.rearrange("(i p) n -> p i n", p=128)
        mx3 = mixT.rearrange("(kc p) t -> p kc t", p=128)
        wsi = [0]

        def rms_to_xn(src, nrow):
            LD("sp", NWB, NWB[:], nws[nrow:nrow + 1, :].partition_broadcast(128))
            ACT(junk[:], src[:], AF.Square, [src], [junk, st2], accum_out=st2[:, 0:1])
            ACT(st2[:, 1:2], st2[:, 0:1], AF.Sqrt, [st2], [st2], scale=1.0 / D, bias=EPS)
            em.op("dve", lambda e: e.reciprocal(out=st2[:, 1:2], in_=st2[:, 1:2]), [st2], [st2])
            STT(xn[:], src[:], st2[:, 1:2], NWB[:], ALU.mult, ALU.mult, [src, st2, NWB], [xn])
            for g in range(8):
                for q in range(4):
                    kc = g * 4 + q
                    f = em.op if q == 0 else em.op_partial_write
                    f("pe", lambda e, kc=kc, q=q: e.transpose(pTr[:, q, :], xn[:, kc * 128:(kc + 1) * 128], idb[:]), [xn, idb], [pTr])
                CP(xnT[:, g * 4:(g + 1) * 4, :], pTr[:], [pTr], [xnT], eng=("act" if g % 2 else "dve"), partial=(g > 0))

        def stream_w(w3, c0, width=512):
            ws = WS[0]
            wsi[0] += 1
            for g in range(4):
                LD("pool", ws, ws[:, g * 8:(g + 1) * 8, 0:width], w3[:, g * 8:(g + 1) * 8, c0:c0 + width], partial=(g > 0))
            return ws

        for tl in range(NTL):
            r0 = tl * 128
            rs_ = slice(r0, r0 + 128)
            LD("sp", xt, xt[:], x_tok[rs_, :])
            LD("pool", mT, mT[:], mx3[:, :, rs_])
            for cg in range(8):
                ws = stream_w(wo3, cg * 512)
                ps = pA[cg % 2]
                for kc in range(KC):
                    MM(ps[:], mT[:, kc, :], ws[:, kc, :], [mT, ws], [ps], start=(kc == 0), stop=(kc == KC - 1))
                TT(h[:, cg * 512:(cg + 1) * 512], xt[:, cg * 512:(cg + 1) * 512], ps[:], ALU.add, [xt, ps], [h], partial=(cg > 0))
            rms_to_xn(h, 0)
            for hp in range(16):
                wq = WQ[0]
                for g in range(4):
                    LD("pool", wq, wq[:, g * 8:(g + 1) * 8, :], wq3[:, g * 8:(g + 1) * 8, hp * 128:(hp + 1) * 128], partial=(g > 0))
                pq = pQ[hp % 2]
                for kc in range(KC):
                    MM(pq[:], wq[:, kc, :], xnT[:, kc, :], [wq, xnT], [pq], start=(kc == 0), stop=(kc == KC - 1))
                CP(qT[:, hp, :], pq[:], [pq], [qT], eng="act", partial=(hp > 0))
            for hp in range(16):
                pq = pQ[hp % 2]
                MM(pq[:], qT[:, hp, :], kT[:, hp, :], [qT, kT], [pq])
                CP(S[:, hp, :], pq[:], [pq], [S], eng=("act" if hp % 2 else "dve"), partial=(hp > 0))
            for hh in range(8):
                s1 = S[:, 2 * hh, :]
                s2 = S[:, 2 * hh + 1, :]
                for (src, tt_) in ((s1, t1), (s2, t2)):
                    em.op("dve", lambda e, src=src, tt_=tt_: e.max(out=tt_[:, 0:8], in_=src), [S], [tt_])
                    em.op("dve", lambda e, src=src, tt_=tt_: e.match_replace(out=wk[:, 0:128], in_to_replace=tt_[:, 0:8], in_values=src, imm_value=-1e30), [S, tt_], [wk])
                    em.op_partial_write("dve", lambda e, tt_=tt_: e.max(out=tt_[:, 8:16], in_=wk[:, 0:128]), [wk], [tt_])
                for a in range(16):
                    TS(cand[:, a, :], t2[:], t1[:, a:a + 1], None, ALU.add, None, [t1, t2], [cand], partial=(a > 0))
                cflat = cand[:].rearrange("p a b -> p (a b)")
                em.op("dve", lambda e, cflat=cflat: e.max(out=ct[:, 0:8], in_=cflat), [cand], [ct])
                em.op("dve", lambda e, cflat=cflat: e.match_replace(out=wk[:], in_to_replace=ct[:, 0:8], in_values=cflat, imm_value=-1e30), [cand, ct], [wk])
                em.op_partial_write("dve", lambda e: e.max(out=ct[:, 8:16], in_=wk[:]), [wk], [ct])
                TS(sc[:, 0:1], ct[:, 0:1], -1.0, None, ALU.mult, None, [ct], [sc])
                ACT(ez[:], ct[:], AF.Exp, [ct, sc], [ez, sc], bias=sc[:, 0:1], accum_out=sc[:, 1:2])
                em.op("dve", lambda e: e.reciprocal(out=sc[:, 2:3], in_=sc[:, 1:2]), [sc], [sc])
                TS(sc[:, 3:4], t1[:, 0:1], -1.0, None, ALU.mult, None, [t1, sc], [sc])
                TS(sc[:, 4:5], t2[:, 0:1], -1.0, None, ALU.mult, None, [t2, sc], [sc])
                ACTp(E1Z[:, hh, :], s1, AF.Exp, [S, sc], [E1Z], bias=sc[:, 3:4])
                TS(E1Z[:, hh, :], E1Z[:, hh, :], sc[:, 2:3], None, ALU.mult, None, [E1Z, sc], [E1Z], partial=True)
                ACTp(E2[:, hh, :], s2, AF.Exp, [S, sc], [E2], bias=sc[:, 4:5])
                TS(TH[:, hh, :], s1, -1.0, ct[:, 15:16], ALU.mult, ALU.add, [S, ct], [TH], partial=True)
            it = 0
            for i in range(128):
                ut = UTt[i % 2]
                for g in range(4):
                    LD("pool", ut, ut[:, g * 8:(g + 1) * 8, :], ut3[:, g * 8:(g + 1) * 8, i * 128:(i + 1) * 128], partial=(g > 0))
                pg, ph = pG[i % 2], pH[i % 2]
                for hh in range(8):
                    g_, gh_ = gm[it % 2], gh[it % 2]
                    it += 1
                    STT(g_[:], S[:, 2 * hh + 1, :], TH[:, hh, i:i + 1], E2[:, hh, :], ALU.is_ge, ALU.mult, [S, TH, E2], [g_])
                    ACT(gh_[:], g_[:], AF.Copy, [g_, E1Z], [gh_], scale=E1Z[:, hh, i:i + 1])
                    f = MM if hh == 0 else MMp
                    f(pg[:], gh_[:], idb[:], [gh_, idb], [pg], start=(hh == 0), stop=(hh == 7))
                for kc in range(KC):
                    f = MM if kc == 0 else MMp
                    f(ph[:], ut[:, kc, :], xnT[:, kc, :], [ut, xnT], [ph], start=(kc == 0), stop=(kc == KC - 1))
                ge = gel[i % 2]
                ACT(ge[:], ph[:], AF.Gelu, [ph], [ge])
                TT(WT[:, i, :], ge[:], pg[:], ALU.mult, [ge, pg], [WT], partial=(i > 0))
            for cg in range(8):
                ps = pA[cg % 2]
                for ic in range(8):
                    vc = WS[0]
                    LD("pool", vc, vc[:, 0:16, :], v3[:, ic * 16:(ic + 1) * 16, cg * 512:(cg + 1) * 512])
                    for q in range(16):
                        i = ic * 16 + q
                        f = MM if i == 0 else MMp
                        f(ps[:], WT[:, i, :], vc[:, q, :], [WT, vc], [ps], start=(i == 0), stop=(i == 127))
                TT(h[:, cg * 512:(cg + 1) * 512], h[:, cg * 512:(cg + 1) * 512], ps[:], ALU.add, [h, ps], [h], partial=True)
            rms_to_xn(h, 1)
            LD("sp", pt, pt[:], p_tok[rs_, :])
            CP(ptb[:], pt[:], [pt], [ptb])
            for q in range(2):
                f = em.op if q == 0 else em.op_partial_write
                f("pe", lambda e, q=q: e.transpose(pTr[:, q, :], ptb[:, q * 128:(q + 1) * 128], idb[:]), [ptb, idb], [pTr])
            CP(pTT[:], pTr[:, 0:2, :], [pTr], [pTT])
            for cg in range(8):
                ps = pA[cg % 2]
                for q in range(2):
                    f = MM if q == 0 else MMp
                    f(ps[:], pTT[:, q, :], WP[:, q, cg * 512:(cg + 1) * 512], [pTT, WP], [ps], start=(q == 0), stop=(q == 1))
                CP(emb[:, cg * 512:(cg + 1) * 512], ps[:], [ps], [emb], eng="act", partial=(cg > 0))
            LD("sp", NWB, NWB[:], nws[2:3, :].partition_broadcast(128))
            ACT(junk[:], emb[:], AF.Square, [emb], [junk, st2], accum_out=st2[:, 2:3])
            ACT(st2[:, 3:4], st2[:, 2:3], AF.Sqrt, [st2], [st2], scale=1.0 / D, bias=EPS)
            em.op("dve", lambda e: e.reciprocal(out=st2[:, 3:4], in_=st2[:, 3:4]), [st2], [st2])
            STT(emb[:], emb[:], st2[:, 3:4], NWB[:], ALU.mult, ALU.mult, [emb, st2, NWB], [emb])
            for cg in range(8):
                ws = stream_w(wg3, cg * 512)
                ps = pA[cg % 2]
                for kc in range(KC):
                    f = MM if kc == 0 else MMp
                    f(ps[:], xnT[:, kc, :], ws[:, kc, :], [xnT, ws], [ps], start=(kc == 0), stop=(kc == KC - 1))
                ACT(gate[:], ps[:], AF.Sigmoid, [ps], [gate])
                TT(gate[:], gate[:], emb[:, cg * 512:(cg + 1) * 512], ALU.mult, [gate, emb], [gate])
                TT(h[:, cg * 512:(cg + 1) * 512], h[:, cg * 512:(cg + 1) * 512], gate[:], ALU.add, [h, gate], [h], partial=True)
            LD("sp", NWB, NWB[:], nws[3:4, :].partition_broadcast(128))
            ACT(junk[:], h[:], AF.Square, [h], [junk, st2], accum_out=st2[:, 0:1])
            ACT(st2[:, 1:2], st2[:, 0:1], AF.Sqrt, [st2], [st2], scale=1.0 / D, bias=EPS)
            em.op("dve", lambda e: e.reciprocal(out=st2[:, 1:2], in_=st2[:, 1:2]), [st2], [st2])
            STT(xt[:], h[:], st2[:, 1:2], NWB[:], ALU.mult, ALU.mult, [h, st2, NWB], [xt])
            finals.append(em.op("sp", lambda e, rs_=rs_: e.dma_start(out=y[rs_, :], in_=xt[:]), [xt], [], dma_key="xt_st"))
    em.emit(final_wait_ops=[finals[-1]])
    print("P2 ops:", em.counts, "sems:", em.n_sems)
    return nc


def prep_p2(inp, mix_all, L, r):
    f32 = np.float32
    TC = L // 8
    ts = slice(r * TC, (r + 1) * TC)
    perm = []
    for c in range(8):
        for g in range(2):
            for j in range(2):
                hh = 2 * c + j
                perm.append(np.arange(g * 2048 + hh * 128, g * 2048 + (hh + 1) * 128))
    perm = np.concatenate(perm)
    sk = np.asarray(inp["peer_sub_keys"], f32)[0]
    keysT = np.ascontiguousarray(sk.reshape(16, 128, 128).transpose(2, 0, 1))
    nws = np.stack([np.asarray(inp[k], f32).reshape(-1) for k in ("ffn_norm_w", "ple_norm_w", "ple_post_norm_w", "final_norm_w")])
    return dict(
        x_tok=np.ascontiguousarray(np.asarray(inp["x"], f32)[0, ts]),
        p_tok=np.ascontiguousarray(np.asarray(inp["p"], f32)[0, 0, ts]),
        mixT=np.ascontiguousarray(mix_all[:, ts]),
        w_out=np.ascontiguousarray(np.asarray(inp["w_out"], f32)[0][perm]),
        w_q=np.asarray(inp["peer_w_query"], f32)[0],
        keysT=keysT,
        UT=np.ascontiguousarray(np.asarray(inp["peer_u"], f32)[0].T),
        V=np.asarray(inp["peer_v"], f32)[0],
        w_gate=np.asarray(inp["ple_w_gate"], f32)[0],
        w_proj=np.asarray(inp["ple_w_proj"], f32)[0],
        nws=np.ascontiguousarray(nws),
        ident=np.eye(128, dtype=f32),
    )


def kernel(**inputs):
    from concourse.bass_utils import run_bass_kernel_spmd
    L = int(np.asarray(inputs["x"]).shape[1])
    nc1 = build_p1(L)
    maps1 = [prep_p1(inputs, L, r) for r in range(8)]
    res1 = run_bass_kernel_spmd(nc1, maps1, core_ids=list(range(8)))
    mix_all = np.concatenate([res1.results[r]["outT"] for r in range(8)], axis=0)
    del maps1
    nc2 = build_p2(L // 8)
    maps2 = [prep_p2(inputs, mix_all, L, r) for r in range(8)]
    res2 = run_bass_kernel_spmd(nc2, maps2, core_ids=list(range(8)))
    out = np.concatenate([res2.results[r]["y"] for r in range(8)], axis=0)
    return out.reshape(1, L, 4096).astype(np.float32)
```

```python
import numpy as np, os
import concourse.bass as bass
import concourse.mybir as mybir

F32 = mybir.dt.float32
BF16 = mybir.dt.bfloat16
ALU = mybir.AluOpType
AF = mybir.ActivationFunctionType
AX = mybir.AxisListType

SEM_CAP = int(os.environ.get("SEM_CAP", "20000"))


class Buf:
    def __init__(self, ap, name):
        self.ap = ap
        self.name = name
        self.writers = []
        self.readers = []

    def __getitem__(self, idx):
        return self.ap[idx]


class Op:
    __slots__ = ("eng", "fn", "deps", "id", "is_dma", "signals", "sig_idx", "dma_sem", "dma_val", "stage", "phys")

    def __init__(self, eng, fn, deps, id_, is_dma):
        self.eng, self.fn, self.deps, self.id, self.is_dma = eng, fn, deps, id_, is_dma
        self.signals = False
        self.sig_idx = -1
        self.dma_sem = None
        self.dma_val = 0
        self.stage = 0
        self.phys = -1


class Emitter:
    ENGS = ("pe", "act", "dve", "pool", "sp")

    def __init__(self, nc, same_engine_sync=True):
        self.nc = nc
        self.ops = []
        self.same_engine_sync = same_engine_sync
        self.dma_groups = {}

    def op(self, eng, fn, reads=(), writes=(), dma_key=None):
        deps = set()
        for b in reads:
            deps.update(b.writers)
        for b in writes:
            deps.update(b.writers)
            deps.update(b.readers)
        oid = len(self.ops)
        o = Op(eng, fn, deps, oid, dma_key is not None)
        self.ops.append(o)
        if dma_key is not None:
            self.dma_groups.setdefault(dma_key, []).append(oid)
            o.dma_sem = dma_key
        for b in reads:
            b.readers.append(oid)
        for b in writes:
            b.writers = [oid]
            b.readers = []
        return oid

    def op_partial_write(self, eng, fn, reads=(), writes=(), dma_key=None):
        deps = set()
        for b in reads:
            deps.update(b.writers)
        for b in writes:
            deps.update(b.readers)
        oid = len(self.ops)
        o = Op(eng, fn, deps, oid, dma_key is not None)
        self.ops.append(o)
        if dma_key is not None:
            self.dma_groups.setdefault(dma_key, []).append(oid)
            o.dma_sem = dma_key
        for b in reads:
            b.readers.append(oid)
        for b in writes:
            b.writers = b.writers + [oid]
        return oid

    def emit(self, final_wait_ops=()):
        nc = self.nc
        ops = self.ops
        for o in ops:
            for d in o.deps:
                t = ops[d]
                if t.is_dma:
                    continue
                if t.eng != o.eng or self.same_engine_sync or o.is_dma:
                    t.signals = True
        nsig = {e: 0 for e in self.ENGS}
        for o in ops:
            if o.is_dma:
                continue
            if o.signals:
                o.sig_idx = nsig[o.eng]
                nsig[o.eng] += 1
        phys = {}
        per_stage = {}
        for o in ops:
            if o.is_dma:
                k = (o.stage, o.dma_sem)
                if k not in phys:
                    idx = per_stage.get(o.stage, 0)
                    phys[k] = idx
                    per_stage[o.stage] = idx + 1
                o.phys = phys[k]
        nphys = max(per_stage.values()) if per_stage else 0
        cum = [0] * nphys
        for o in ops:
            if o.is_dma:
                cum[o.phys] += 16
                o.dma_val = cum[o.phys]
        esems = {}
        for e in self.ENGS:
            n_ep = (nsig[e] + SEM_CAP - 1) // SEM_CAP
            esems[e] = [nc.alloc_semaphore(name=f"e_{e}_{i}") for i in range(n_ep)]
        dsems = [nc.alloc_semaphore(name=f"d_{i}") for i in range(nphys)]
        self.n_sems = sum(len(v) for v in esems.values()) + len(dsems)

        def target(t):
            if t.is_dma:
                return (dsems[t.phys], t.dma_val, ("d", t.phys))
            ep, v = divmod(t.sig_idx, SEM_CAP)
            return (esems[t.eng][ep], v + 1, ("e", t.eng, ep))

        progs = {e: [] for e in self.ENGS}
        seen = {e: {} for e in self.ENGS}
        for o in ops:
            waits = {}
            for d in o.deps:
                t = ops[d]
                if (not t.is_dma) and t.eng == o.eng and not (self.same_engine_sync or o.is_dma):
                    continue
                sem, val, k = target(t)
                if seen[o.eng].get(k, 0) >= val:
                    continue
                if k not in waits or waits[k][1] < val:
                    waits[k] = (sem, val)
            for k, (sem, val) in waits.items():
                seen[o.eng][k] = val
            progs[o.eng].append((list(waits.values()), o))
        fin = []
        for oid in final_wait_ops:
            sem, val, k = target(ops[oid])
            fin.append((sem, val))

        handles = {}

        def run(ename, eng):
            for waits, o in progs[ename]:
                for sem, val in waits:
                    eng.wait_ge(sem, val)
                ins = o.fn(eng)
                if o.is_dma:
                    ins.then_inc(dsems[o.phys], 16)
                elif o.signals:
                    ep = o.sig_idx // SEM_CAP
                    ins.then_inc(esems[ename][ep], 1)
            if ename == "sp":
                for sem, val in fin:
                    eng.wait_ge(sem, val)

        with nc.Block() as block:
            @block.tensor
            def _(e):
                run("pe", e)

            @block.scalar
            def _(e):
                run("act", e)

            @block.vector
            def _(e):
                run("dve", e)

            @block.gpsimd
            def _(e):
                run("pool", e)

            @block.sync
            def _(e):
                run("sp", e)
        self.counts = {e: len(progs[e]) for e in self.ENGS}


def _barrier(self):
    last = {}
    for o in self.ops:
        if o.is_dma:
            last[("d", o.dma_sem)] = o.id
        else:
            last[("e", o.eng)] = o.id
    self._bar_deps = set(last.values())
    self._stage = getattr(self, "_stage", 0) + 1


_orig_op = Emitter.op
_orig_opp = Emitter.op_partial_write


def _op(self, eng, fn, reads=(), writes=(), dma_key=None):
    oid = _orig_op(self, eng, fn, reads, writes, dma_key)
    self.ops[oid].stage = getattr(self, "_stage", 0)
    bd = getattr(self, "_bar_deps", None)
    if bd:
        self.ops[oid].deps.update(bd)
    return oid


def _opp(self, eng, fn, reads=(), writes=(), dma_key=None):
    oid = _orig_opp(self, eng, fn, reads, writes, dma_key)
    self.ops[oid].stage = getattr(self, "_stage", 0)
    bd = getattr(self, "_bar_deps", None)
    if bd:
        self.ops[oid].deps.update(bd)
    return oid


Emitter.barrier = _barrier
Emitter.op = _op
Emitter.op_partial_write = _opp


import numpy as np, os
from contextlib import ExitStack
import concourse.bass as bass
import concourse.mybir as mybir

D = 4096
KC = D // 128
NTOK = 1544
NFEAT = 768
EPS = 1e-6
C_ID, C_ONES, C_HMF, C_HMB, C_CIND, C_HKF, C_HKB, C_UF, C_UB, C_MBI, C_MBIT, C_MBS, C_MBST, C_END = (
    0, 128, 256, 384, 512, 520, 584, 648, 776, 904, 1032, 1160, 1288, 1416)


def make_consts():
    c = np.zeros((128, C_END), np.float32)
    p = np.arange(128)[:, None]
    f = np.arange(128)[None, :]
    same = (p // 64) == (f // 64)
    c[:, C_ID:C_ID + 128] = (p == f)
    c[:, C_ONES:C_ONES + 128] = 1.0
    c[:, C_HMF:C_HMF + 128] = same & (p > f)
    c[:, C_HMB:C_HMB + 128] = same & (p < f)
    c[:, C_CIND] = (np.arange(128) < 64)
    c[:, C_CIND + 1] = (np.arange(128) >= 64)
    f64 = np.arange(64)[None, :]
    c[:, C_HKF:C_HKF + 64] = (p <= f64)
    c[:, C_HKB:C_HKB + 64] = (p >= f64)
    c[:, C_UF:C_UF + 128] = (p <= f)
    c[:, C_UB:C_UB + 128] = (p >= f)
    NEG = -30000.0
    c[:, C_MBI:C_MBI + 128] = np.where(p >= f, 0.0, NEG)
    c[:, C_MBIT:C_MBIT + 128] = np.where(f >= p, 0.0, NEG)
    c[:, C_MBS:C_MBS + 128] = np.where(p > f, 0.0, NEG)
    c[:, C_MBST:C_MBST + 128] = np.where(f > p, 0.0, NEG)
    return c


class _Stop(Exception):
    pass


def build_p1(L, debug=False, stop_after=99):
    nc = bass.Bass("TRN2", target_bir_lowering=False)
    NB = L // 512
    NT = L // 128
    NCH = L // 64
    dt_in = lambda n, s: nc.dram_tensor(n, s, F32, kind="ExternalInput").ap()
    xT = dt_in("xT", [D, L])
    w_tok = dt_in("w_tok", [D, NTOK])
    w_feat = dt_in("w_feat", [D, NFEAT])
    consts_d = dt_in("consts", [128, C_END])
    anw = dt_in("anw", [128, KC])
    lbp = dt_in("lbp", [1, 2 * 2 * 2 * 128])
    convw = dt_in("convw", [6, 128, 5])
    gdp = dt_in("gdp", [1, 8])
    nws = dt_in("nws", [1, 256])
    outT = nc.dram_tensor("outT", [512, L], F32, kind="ExternalOutput").ap()

    em = Emitter(nc)
    cnt = [0]

    def dram(name, shape, dt):
        return nc.dram_tensor(name, shape, dt, kind="Internal").ap()

    regs = {}

    def R(name, key):
        k = (name, key)
        if k not in regs:
            regs[k] = Buf(None, f"{name}_{key}")
        return regs[k]

    def sbt(es, name, shape, dt):
        h = es.enter_context(nc.sbuf_tensor(name, shape, dt))
        return Buf(h.ap() if hasattr(h, "ap") else h, name)

    def pst(es, name, shape, dt):
        h = es.enter_context(nc.psum_tensor(name, shape, dt))
        return Buf(h.ap() if hasattr(h, "ap") else h, name)

    def ACT(out, in_, func, reads, writes, **kw):
        return em.op("act", lambda e: e.activation(out=out, in_=in_, func=func, **kw), reads, writes)

    def MM(out, lhsT, rhs, reads, writes, start=True, stop=True):
        return em.op("pe", lambda e: e.matmul(out, lhsT=lhsT, rhs=rhs, start=start, stop=stop), reads, writes)

    def TR(out, in_, ident, reads, writes):
        return em.op("pe", lambda e: e.transpose(out, in_, ident), reads, writes)

    def TS(out, in0, s1, s2, op0, op1, reads, writes, eng="dve"):
        if op1 is None:
            return em.op(eng, lambda e: e.tensor_scalar(out=out, in0=in0, scalar1=s1, scalar2=None, op0=op0), reads, writes)
        return em.op(eng, lambda e: e.tensor_scalar(out=out, in0=in0, scalar1=s1, scalar2=s2, op0=op0, op1=op1), reads, writes)

    def TT(out, in0, in1, op, reads, writes, eng="dve"):
        return em.op(eng, lambda e: e.tensor_tensor(out=out, in0=in0, in1=in1, op=op), reads, writes)

    def STT(out, in0, scalar, in1, op0, op1, reads, writes, eng="dve"):
        return em.op(eng, lambda e: e.scalar_tensor_tensor(out=out, in0=in0, scalar=scalar, in1=in1, op0=op0, op1=op1), reads, writes)

    def CP(out, in_, reads, writes, eng="dve"):
        if eng == "act":
            return ACT(out, in_, AF.Copy, reads, writes)
        return em.op(eng, lambda e: e.tensor_copy(out=out, in_=in_), reads, writes)

    def MEMSET(out, val, writes, eng="dve"):
        return em.op(eng, lambda e: e.memset(out, val), (), writes)

    def LD(eng, buf, out_ap, in_ap, rbufs=(), partial=False):
        f = em.op_partial_write if partial else em.op
        return f(eng, lambda e: e.dma_start(out=out_ap, in_=in_ap), list(rbufs), [buf], dma_key=buf.name + "_ld")

    def ST(eng, out_ap, dbufs, buf, in_ap):
        return em.op_partial_write(eng, lambda e: e.dma_start(out=out_ap, in_=in_ap), [buf], list(dbufs), dma_key=buf.name + "_st")

    xnT = dram("xnT", [D, L], BF16)
    raw_tok = dram("raw_tok", [L, NTOK], F32)
    rawT = dram("rawT", [NFEAT, L], F32)
    hg_v = [dram(f"hg_v{j}", [L, 128], BF16) for j in range(2)]
    hg_kh = [[dram(f"hg_kh{j}{d}", [L, 128], BF16) for d in range(2)] for j in range(2)]
    hg_khT = [[dram(f"hg_khT{j}{d}", [128, L], BF16) for d in range(2)] for j in range(2)]
    hg_qhT = [[dram(f"hg_qhT{j}{d}", [128, L], BF16) for d in range(2)] for j in range(2)]
    o_hg = [[dram(f"o_hg{j}{d}", [L, 128], F32) for d in range(2)] for j in range(2)]
    gd_qT = [dram(f"gd_qT{j}", [128, L], BF16) for j in range(2)]
    gd_kT = [dram(f"gd_kT{j}", [128, L], BF16) for j in range(2)]
    gd_k = [dram(f"gd_k{j}", [L, 128], BF16) for j in range(2)]
    gd_v = [dram(f"gd_v{j}", [L, 128], BF16) for j in range(2)]
    gd_u = [[dram(f"gd_u{j}{d}", [L, 128], F32) for d in range(2)] for j in range(2)]
    gd_wT = [[dram(f"gd_wT{j}{d}", [128, L], BF16) for d in range(2)] for j in range(2)]
    gd_qk = [[dram(f"gd_qk{j}{d}", [L, 128], BF16) for d in range(2)] for j in range(2)]
    gd_kd = [[dram(f"gd_kd{j}{d}", [L, 128], BF16) for d in range(2)] for j in range(2)]
    o_gd = [[dram(f"o_gd{j}{d}", [L, 128], F32) for d in range(2)] for j in range(2)]

    finals = []
    try:
      with ExitStack() as es0:
          cst = sbt(es0, "cst", [128, C_END], F32)
          idb = sbt(es0, "idb", [128, 128], BF16)
          onb = sbt(es0, "onb", [128, 128], BF16)
          nwa = sbt(es0, "nwa", [128, KC], F32)
          lbt = sbt(es0, "lbt", [128, 8, 128], F32)
          LB = sbt(es0, "LB", [128, 4, 128], F32)
          OML = sbt(es0, "OML", [128, 4, 128], F32)
          NW = sbt(es0, "NW", [128, 256], F32)
          gdpt = sbt(es0, "gdpt", [128, 8], F32)
          NEGA = sbt(es0, "NEGA", [128, 4], F32)
          CW = sbt(es0, "CW", [128, 6, 5], F32)
          GDEC = sbt(es0, "GDEC", [128, 4, NCH + 2], F32)
          GALL = sbt(es0, "GALL", [128, NT, 4], F32)
          BALL = sbt(es0, "BALL", [128, NT, 4], F32)
          NBALL = sbt(es0, "NBALL", [128, NT, 4], F32)
          EG = sbt(es0, "EG", [128, 4, NT], F32)
          GL = sbt(es0, "GL", [128, 4, NT], F32)

          LD("sp", cst, cst[:], consts_d[:, :])
          LD("pool", idb, idb[:], consts_d[:, C_ID:C_ID + 128])
          LD("pool", onb, onb[:], consts_d[:, C_ONES:C_ONES + 128])
          LD("sp", nwa, nwa[:], anw[:, :])
          LD("sp", lbt, lbt[:].rearrange("p a b -> p (a b)"), lbp.partition_broadcast(128))
          LD("sp", NW, NW[:], nws.partition_broadcast(128))
          LD("sp", gdpt, gdpt[:], gdp.partition_broadcast(128))
          LD("sp", CW, CW[:], convw.rearrange("a p t -> p a t"))
          TT(LB[:], lbt[:, 0:4, :], lbt[:, 4:8, :], ALU.subtract, [lbt], [LB])
          ACT(LB[:], LB[:], AF.Sigmoid, [LB], [LB])
          TS(OML[:], LB[:], -1.0, 1.0, ALU.mult, ALU.add, [LB], [OML])
          ACT(NEGA[:], gdpt[:, 0:4], AF.Exp, [gdpt], [NEGA])
          TS(NEGA[:], NEGA[:], -1.0, None, ALU.mult, None, [NEGA], [NEGA])
          MEMSET(GDEC[:], 1.0, [GDEC])
          ident = cst[:, C_ID:C_ID + 128]
          ones_f = cst[:, C_ONES:C_ONES + 128]

          with ExitStack() as es:
              XB = [sbt(es, f"XB{i}", [128, KC, 512], F32) for i in range(1)]
              XN = [sbt(es, f"XN{i}", [128, KC, 512], BF16) for i in range(2)]
              SQ = [sbt(es, f"SQ{i}", [128, 512], BF16) for i in range(3)]
              rst = [sbt(es, f"rst{i}", [128, 512], F32) for i in range(2)]
              pss = [pst(es, f"pss{i}", [128, 512], F32) for i in range(2)]
              xT3 = xT.rearrange("(kc p) t -> p kc t", p=128)
              xn3 = xnT.rearrange("(kc p) t -> p kc t", p=128)
              for b in range(NB):
                  xb, xn, ps, rs = XB[0], XN[b % 2], pss[b % 2], rst[b % 2]
                  ts_ = slice(b * 512, (b + 1) * 512)
                  for g in range(4):
                      LD("sp", xb, xb[:, g * 8:(g + 1) * 8, :], xT3[:, g * 8:(g + 1) * 8, ts_], partial=(g > 0))
                  for kc in range(KC):
                      sq = SQ[kc % 3]
                      ACT(sq[:], xb[:, kc, :], AF.Square, [xb], [sq])
                      MM(ps[:], onb[:], sq[:], [onb, sq], [ps], start=(kc == 0), stop=(kc == KC - 1))
                  ACT(rs[:], ps[:], AF.Sqrt, [ps], [rs], scale=1.0 / D, bias=EPS)
                  em.op("dve", lambda e, rs=rs: e.reciprocal(out=rs[:], in_=rs[:]), [rs], [rs])
                  for kc in range(KC):
                      f = em.op if kc == 0 else em.op_partial_write
                      f("dve", lambda e, xn=xn, xb=xb, kc=kc, rs=rs: e.scalar_tensor_tensor(
                          out=xn[:, kc, :], in0=xb[:, kc, :], scalar=nwa[:, kc:kc + 1], in1=rs[:],
                          op0=ALU.mult, op1=ALU.mult), [xb, nwa, rs], [xn])
                  for g in range(4):
                      ST("sp", xn3[:, g * 8:(g + 1) * 8, ts_], [R("xnT", b)], xn, xn[:, g * 8:(g + 1) * 8, :])
          em.barrier()
          if stop_after <= 1:
              raise _Stop()

          with ExitStack() as es:
              WB = sbt(es, "WB", [128, KC, NTOK], BF16)
              XN = [sbt(es, f"XNb{i}", [128, KC, 512], BF16) for i in range(2)]
              stg = [sbt(es, f"stg{i}", [128, NTOK], F32) for i in range(1)]
              pp = [pst(es, f"pp{i}", [128, 512], F32) for i in range(4)]
              w3 = w_tok.rearrange("(kc p) n -> p kc n", p=128)
              for g in range(8):
                  LD("pool", WB, WB[:, g * 4:(g + 1) * 4, :], w3[:, g * 4:(g + 1) * 4, :], partial=(g > 0))
              cgs = [(0, 512), (512, 1024), (1024, 1536), (1536, NTOK)]
              it = 0
              for b in range(NB):
                  xn = XN[b % 2]
                  ts_ = slice(b * 512, (b + 1) * 512)
                  for g in range(4):
                      LD("sp", xn, xn[:, g * 8:(g + 1) * 8, :], xn3[:, g * 8:(g + 1) * 8, ts_], rbufs=[R("xnT", b)], partial=(g > 0))
                  for t in range(4):
                      sg = stg[0]
                      for ci, (c0, c1) in enumerate(cgs):
                          ps = pp[it % 4]
                          it += 1
                          for kc in range(KC):
                              MM(ps[:, 0:c1 - c0], xn[:, kc, t * 128:(t + 1) * 128], WB[:, kc, c0:c1], [xn, WB], [ps],
                                 start=(kc == 0), stop=(kc == KC - 1))
                          f = em.op if ci == 0 else em.op_partial_write
                          if ci % 2 == 0:
                              f("act", lambda e, sg=sg, ps=ps, c0=c0, c1=c1: e.activation(out=sg[:, c0:c1], in_=ps[:, 0:c1 - c0], func=AF.Copy), [ps], [sg])
                          else:
                              f("dve", lambda e, sg=sg, ps=ps, c0=c0, c1=c1: e.tensor_copy(out=sg[:, c0:c1], in_=ps[:, 0:c1 - c0]), [ps], [sg])
                      r0 = b * 512 + t * 128
                      ST("sp", raw_tok[r0:r0 + 128, :], [R("raw_tok", b)], sg, sg[:])
          em.barrier()
          if stop_after <= 2:
              raise _Stop()

          with ExitStack() as es:
              WC = sbt(es, "WC", [128, KC, NFEAT], BF16)
              XN = [sbt(es, f"XNc{i}", [128, KC, 512], BF16) for i in range(2)]
              stg = [sbt(es, f"stgc{i}", [128, 512], F32) for i in range(3)]
              pp = [pst(es, f"ppc{i}", [128, 512], F32) for i in range(4)]
              w3 = w_feat.rearrange("(kc p) n -> p kc n", p=128)
              for g in range(8):
                  LD("pool", WC, WC[:, g * 4:(g + 1) * 4, :], w3[:, g * 4:(g + 1) * 4, :], partial=(g > 0))
              it = 0
              for b in range(NB):
                  xn = XN[b % 2]
                  ts_ = slice(b * 512, (b + 1) * 512)
                  for g in range(4):
                      LD("sp", xn, xn[:, g * 8:(g + 1) * 8, :], xn3[:, g * 8:(g + 1) * 8, ts_], rbufs=[R("xnT", b)], partial=(g > 0))
                  for m in range(6):
                      ps = pp[it % 4]
                      sg = stg[it % 3]
                      it += 1
                      for kc in range(KC):
                          MM(ps[:], WC[:, kc, m * 128:(m + 1) * 128], xn[:, kc, :], [WC, xn], [ps], start=(kc == 0), stop=(kc == KC - 1))
                      CP(sg[:], ps[:], [ps], [sg], eng=("act" if m % 2 else "dve"))
                      ST("sp", rawT[m * 128:(m + 1) * 128, ts_], [R("rawT", b)], sg, sg[:])
          em.barrier()
          if stop_after <= 3:
              raise _Stop()

          with ExitStack() as es:
              RT = [sbt(es, f"RT{i}", [128, NTOK], F32) for i in range(2)]
              qs = sbt(es, "qs", [128, 128], F32)
              sgm = sbt(es, "sgm", [128, 128], F32)
              ff = sbt(es, "ff", [128, 128], F32)
              lf = sbt(es, "lf", [128, 128], F32)
              kk = sbt(es, "kk", [128, 128], F32)
              eD = sbt(es, "eD", [128, 128], F32)
              enD = sbt(es, "enD", [128, 128], F32)
              kh = [sbt(es, f"kh{i}", [128, 128], BF16) for i in range(2)]
              qh = sbt(es, "qh", [128, 128], BF16)
              khT = [sbt(es, f"khT{i}", [128, 128], BF16) for i in range(2)]
              qhT = [sbt(es, f"qhT{i}", [128, 128], BF16) for i in range(2)]
              vb = [sbt(es, f"vb{i}", [128, 128], BF16) for i in range(2)]
              gt = sbt(es, "gt", [128, 4], F32)
              pD = pst(es, "pD", [128, 128], F32)
              pG = pst(es, "pG", [128, 2], F32)
              pT1 = pst(es, "pT1", [128, 128], BF16)
              pT2 = pst(es, "pT2", [128, 128], BF16)
              it = 0
              for t in range(NT):
                  rt = RT[t % 2]
                  b = t // 4
                  LD("sp", rt, rt[:], raw_tok[t * 128:(t + 1) * 128, :], rbufs=[R("raw_tok", b)])
                  tsl = slice(t * 128, (t + 1) * 128)
                  TT(gt[:], rt[:, 1280:1284], gdpt[:, 4:8], ALU.add, [rt, gdpt], [gt])
                  ACT(gt[:], gt[:], AF.Exp, [gt], [gt])
                  ACT(gt[:], gt[:], AF.Ln, [gt], [gt], bias=1.0)
                  em.op_partial_write("dve", lambda e, t=t: e.tensor_tensor(out=GALL[:, t, :], in0=gt[:], in1=NEGA[:], op=ALU.mult), [gt, NEGA], [GALL])
                  em.op_partial_write("act", lambda e, t=t, rt=rt: e.activation(out=BALL[:, t, :], in_=rt[:, 1284:1288], func=AF.Sigmoid), [rt], [BALL])
                  for j in range(2):
                      ACT(qs[:], rt[:, j * 128:(j + 1) * 128], AF.Silu, [rt], [qs])
                      v_ = vb[j]
                      CP(v_[:], rt[:, 768 + j * 128:768 + (j + 1) * 128], [rt], [v_])
                      ST("sp", hg_v[j][tsl, :], [R(f"hg_v{j}", b)], v_, v_[:])
                      for d in range(2):
                          zc = 256 + d * 256 + j * 128
                          dj = d * 2 + j
                          khb, khTb, qhTb = kh[it % 2], khT[it % 2], qhT[it % 2]
                          it += 1
                          ACT(sgm[:], rt[:, zc:zc + 128], AF.Sigmoid, [rt], [sgm])
                          TT(ff[:], sgm[:], OML[:, dj, :], ALU.mult, [sgm, OML], [ff])
                          TT(ff[:], ff[:], LB[:, dj, :], ALU.add, [ff, LB], [ff])
                          ACT(lf[:], ff[:], AF.Ln, [ff], [lf])
                          TS(kk[:], ff[:], -1.0, 1.0, ALU.mult, ALU.add, [ff], [kk])
                          mcol = C_HMF if d == 0 else C_HMB
                          MM(pD[:], cst[:, mcol:mcol + 128], lf[:], [cst, lf], [pD])
                          MM(pG[:], lf[:], cst[:, C_CIND:C_CIND + 2], [cst, lf], [pG])
                          em.op_partial_write("act", lambda e, dj=dj, t=t: e.activation(out=GDEC[:, dj, 2 * t:2 * t + 2], in_=pG[:], func=AF.Exp), [pG], [GDEC])
                          ACT(eD[:], pD[:], AF.Exp, [pD], [eD])
                          ACT(enD[:], pD[:], AF.Exp, [pD], [enD], scale=-1.0)
                          TT(khb[:], kk[:], eD[:], ALU.mult, [kk, eD], [khb])
                          STT(qh[:], qs[:], 128 ** -0.5, enD[:], ALU.mult, ALU.mult, [qs, enD], [qh])
                          TR(pT1[:], khb[:], idb[:], [khb, idb], [pT1])
                          TR(pT2[:], qh[:], idb[:], [qh, idb], [pT2])
                          CP(khTb[:], pT1[:], [pT1], [khTb], eng="act")
                          CP(qhTb[:], pT2[:], [pT2], [qhTb], eng="dve")
                          ST("sp", hg_kh[j][d][tsl, :], [R(f"hg_kh{j}{d}", b)], khb, khb[:])
                          ST("sp", hg_khT[j][d][:, tsl], [R(f"hg_khT{j}{d}", b)], khTb, khTb[:])
                          ST("sp", hg_qhT[j][d][:, tsl], [R(f"hg_qhT{j}{d}", b)], qhTb, qhTb[:])
              TS(NBALL[:], BALL[:], -1.0, None, ALU.mult, None, [BALL], [NBALL])
          em.barrier()
          if stop_after <= 4:
              raise _Stop()

          with ExitStack() as es:
              XW = [sbt(es, f"XW{i}", [128, 516], F32) for i in range(2)]
              acc = sbt(es, "acc", [128, 512], F32)
              sl = sbt(es, "sl", [128, 512], F32)
              sq = sbt(es, "sq5", [128, 512], BF16)
              rn = sbt(es, "rn", [128, 512], F32)
              ob = [sbt(es, f"ob{i}", [128, 512], BF16) for i in range(2)]
              tk = [sbt(es, f"tk{i}", [128, 4, 128], BF16) for i in range(2)]
              pS5 = pst(es, "pS5", [128, 512], F32)
              pT5 = pst(es, "pT5", [128, 4, 128], BF16)
              it = 0
              for b in range(NB):
                  for j in range(2):
                      for w in range(3):
                          m = j * 3 + w
                          row0 = w * 256 + j * 128
                          xw = XW[it % 2]
                          obb, tkb = ob[it % 2], tk[it % 2]
                          it += 1
                          lo = b * 512 - 2
                          hi = b * 512 + 514
                          c_lo = 0
                          c_hi = 516
                          if b == 0:
                              lo, c_lo = 0, 2
                          if b == NB - 1:
                              hi, c_hi = L, 514
                          rb = [R("rawT", bb) for bb in (b - 1, b, b + 1) if 0 <= bb < NB]
                          MEMSET(xw[:], 0.0, [xw])
                          LD("sp", xw, xw[:, c_lo:c_hi], rawT[row0:row0 + 128, lo:hi], rbufs=rb, partial=True)
                          TS(acc[:], xw[:, 0:512], CW[:, m, 0:1], None, ALU.mult, None, [xw, CW], [acc])
                          for tp in range(1, 5):
                              STT(acc[:], xw[:, tp:tp + 512], CW[:, m, tp:tp + 1], acc[:], ALU.mult, ALU.add, [xw, CW, acc], [acc])
                          ACT(sl[:], acc[:], AF.Silu, [acc], [sl])
                          ts_ = slice(b * 512, (b + 1) * 512)
                          if w < 2:
                              ACT(sq[:], sl[:], AF.Square, [sl], [sq])
                              MM(pS5[:], onb[:], sq[:], [onb, sq], [pS5])
                              ACT(rn[:], pS5[:], AF.Sqrt, [pS5], [rn], bias=EPS)
                              em.op("dve", lambda e: e.reciprocal(out=rn[:], in_=rn[:]), [rn], [rn])
                              sc = (128 ** -0.5) if w == 0 else 1.0
                              STT(obb[:], sl[:], sc, rn[:], ALU.mult, ALU.mult, [sl, rn], [obb])
                              dst = gd_qT[j] if w == 0 else gd_kT[j]
                              nm = f"gd_qT{j}" if w == 0 else f"gd_kT{j}"
                              ST("sp", dst[:, ts_], [R(nm, b)], obb, obb[:])
                          else:
                              CP(obb[:], sl[:], [sl], [obb])
                          if w >= 1:
                              for q4 in range(4):
                                  f = em.op if q4 == 0 else em.op_partial_write
                                  f("pe", lambda e, q4=q4, obb=obb: e.transpose(pT5[:, q4, :], obb[:, q4 * 128:(q4 + 1) * 128], idb[:]), [obb, idb], [pT5])
                              CP(tkb[:], pT5[:], [pT5], [tkb], eng="act")
                              dst = gd_k[j] if w == 1 else gd_v[j]
                              nm = f"gd_k{j}" if w == 1 else f"gd_v{j}"
                              ST("sp", dst[ts_, :].rearrange("(q p) n -> p q n", p=128), [R(nm, b)], tkb, tkb[:])
          em.barrier()
          if stop_after <= 5:
              raise _Stop()

          with ExitStack() as es:
              kTb = [sbt(es, f"kTb{i}", [128, 512], BF16) for i in range(2)]
              qTb = [sbt(es, f"qTb{i}", [128, 512], BF16) for i in range(2)]
              kt4 = [sbt(es, f"kt4{i}", [128, 4, 128], BF16) for i in range(2)]
              vt4 = [sbt(es, f"vt4{i}", [128, 4, 128], BF16) for i in range(2)]
              gc = sbt(es, "gc", [128, 2], F32)
              sc4 = sbt(es, "sc4", [128, 4], F32)
              dg = sbt(es, "dg", [128, 128], F32)
              t1 = sbt(es, "t1", [128, 128], F32)
              t2 = sbt(es, "t2", [128, 128], F32)
              rels = sbt(es, "rels", [128, 128], F32)
              relT = sbt(es, "relT", [128, 128], F32)
              P = [sbt(es, f"P{i}", [128, 128], F32) for i in range(2)]
              PT = [sbt(es, f"PT{i}", [128, 128], F32) for i in range(2)]
              X = [sbt(es, f"X{i}", [128, 256], F32) for i in range(2)]
              wbf = sbt(es, "wbf", [128, 128], BF16)
              wTs = [sbt(es, f"wTs{i}", [128, 128], BF16) for i in range(2)]
              qks = [sbt(es, f"qks{i}", [128, 128], BF16) for i in range(2)]
              kds = [sbt(es, f"kds{i}", [128, 128], BF16) for i in range(2)]
              us = [sbt(es, f"us{i}", [128, 128], F32) for i in range(2)]
              pg = pst(es, "pg6", [128, 2], F32)
              pR = pst(es, "pR", [128, 128], F32)
              pK = pst(es, "pK", [128, 128], F32)
              pQ = pst(es, "pQ", [128, 128], F32)
              pX = pst(es, "pX", [128, 256], F32)
              pP = pst(es, "pP", [128, 128], F32)
              pPT = pst(es, "pPT", [128, 128], F32)
              pW = pst(es, "pW", [128, 128], BF16)
              it = 0
              for b in range(NB):
                  for j in range(2):
                      kT_, qT_, k4, v4 = kTb[(b * 2 + j) % 2], qTb[(b * 2 + j) % 2], kt4[(b * 2 + j) % 2], vt4[(b * 2 + j) % 2]
                      ts_ = slice(b * 512, (b + 1) * 512)
                      LD("sp", kT_, kT_[:], gd_kT[j][:, ts_], rbufs=[R(f"gd_kT{j}", b)])
                      LD("sp", qT_, qT_[:], gd_qT[j][:, ts_], rbufs=[R(f"gd_qT{j}", b)])
                      LD("sp", k4, k4[:], gd_k[j][ts_, :].rearrange("(q p) n -> p q n", p=128), rbufs=[R(f"gd_k{j}", b)])
                      LD("sp", v4, v4[:], gd_v[j][ts_, :].rearrange("(q p) n -> p q n", p=128), rbufs=[R(f"gd_v{j}", b)])
                      for q4 in range(4):
                          t = b * 4 + q4
                          tsl = slice(t * 128, (t + 1) * 128)
                          cs = slice(q4 * 128, (q4 + 1) * 128)
                          for d in range(2):
                              dj = d * 2 + j
                              i2 = it % 2
                              it += 1
                              ucol = C_UF if d == 0 else C_UB
                              mbs = (C_MBS if d == 0 else C_MBST)
                              mbiT = (C_MBIT if d == 0 else C_MBI)
                              gcol = GALL[:, t, dj:dj + 1]
                              MM(pg[:, 0:1], cst[:, ucol:ucol + 128], gcol, [cst, GALL], [pg])
                              em.op_partial_write("pe", lambda e, gcol=gcol: e.matmul(pg[:, 1:2], lhsT=ones_f, rhs=gcol, start=True, stop=True), [cst, GALL], [pg])
                              CP(gc[:], pg[:], [pg], [gc])
                              em.op_partial_write("act", lambda e, dj=dj, t=t: e.activation(out=EG[:, dj, t:t + 1], in_=gc[:, 0:1], func=AF.Exp), [gc], [EG])
                              em.op_partial_write("act", lambda e, dj=dj, t=t: e.activation(out=GL[:, dj, t:t + 1], in_=gc[:, 1:2], func=AF.Exp), [gc], [GL])
                              ACT(sc4[:, 0:1], gc[:, 0:1], AF.Exp, [gc], [sc4], scale=-1.0, bias=gc[:, 1:2])
                              em.op_partial_write("dve", lambda e, dj=dj, t=t: e.tensor_tensor(out=sc4[:, 1:2], in0=EG[:, dj, t:t + 1], in1=BALL[:, t, dj:dj + 1], op=ALU.mult), [EG, BALL], [sc4])
                              TS(dg[:], ident, gc[:, 0:1], None, ALU.mult, None, [cst, gc], [dg])
                              MM(pR[:], ones_f, dg[:], [cst, dg], [pR])
                              TS(t1[:], pR[:], gc[:, 0:1], -1.0, ALU.subtract, ALU.mult, [pR, gc], [t1])
                              TT(t1[:], t1[:], cst[:, mbs:mbs + 128], ALU.add, [t1, cst], [t1])
                              ACT(rels[:], t1[:], AF.Exp, [t1], [rels])
                              TS(t2[:], pR[:], gc[:, 0:1], None, ALU.subtract, None, [pR, gc], [t2])
                              TT(t2[:], t2[:], cst[:, mbiT:mbiT + 128], ALU.add, [t2, cst], [t2])
                              ACT(relT[:], t2[:], AF.Exp, [t2], [relT])
                              MM(pK[:], kT_[:, cs], kT_[:, cs], [kT_], [pK])
                              p_, pt_ = P[0], PT[0]
                              STT(p_[:], pK[:], NBALL[:, t, dj:dj + 1], rels[:], ALU.mult, ALU.mult, [pK, NBALL, rels], [p_])
                              TR(pPT[:], p_[:], ident, [p_, cst], [pPT])
                              CP(pt_[:], pPT[:], [pPT], [pt_], eng="act")
                              MM(pQ[:], kT_[:, cs], qT_[:, cs], [kT_, qT_], [pQ])
                              qk_ = qks[i2]
                              TT(qk_[:], pQ[:], relT[:], ALU.mult, [pQ, relT], [qk_])
                              ST("sp", gd_qk[j][d][tsl, :], [R(f"gd_qk{j}{d}", b)], qk_, qk_[:])
                              kd_ = kds[i2]
                              TS(kd_[:], k4[:, q4, :], sc4[:, 0:1], None, ALU.mult, None, [k4, sc4], [kd_], eng="pool")
                              ST("sp", gd_kd[j][d][tsl, :], [R(f"gd_kd{j}{d}", b)], kd_, kd_[:])
                              x_ = X[0]
                              TS(x_[:, 0:128], v4[:, q4, :], BALL[:, t, dj:dj + 1], None, ALU.mult, None, [v4, BALL], [x_], eng="pool")
                              em.op_partial_write("pool", lambda e, x_=x_, k4=k4, q4=q4: e.tensor_scalar(out=x_[:, 128:256], in0=k4[:, q4, :], scalar1=sc4[:, 1:2], scalar2=None, op0=ALU.mult), [k4, sc4], [x_])
                              cur = 0
                              for m in range(7):
                                  xs, xd = X[cur], X[1 - cur]
                                  pc, ptc = P[cur], PT[cur]
                                  MM(pX[:], ptc[:], xs[:], [ptc, xs], [pX])
                                  TT(xd[:], xs[:], pX[:], ALU.add, [xs, pX], [xd])
                                  if m < 6:
                                      pn, ptn = P[1 - cur], PT[1 - cur]
                                      MM(pP[:], ptc[:], pc[:], [ptc, pc], [pP])
                                      MM(pPT[:], pc[:], ptc[:], [ptc, pc], [pPT])
                                      CP(pn[:], pP[:], [pP], [pn], eng="act")
                                      CP(ptn[:], pPT[:], [pPT], [ptn], eng="act")
                                  cur = 1 - cur
                              xf = X[cur]
                              u_ = us[i2]
                              CP(u_[:], xf[:, 0:128], [xf], [u_], eng="pool")
                              ST("sp", gd_u[j][d][tsl, :], [R(f"gd_u{j}{d}", b)], u_, u_[:])
                              CP(wbf[:], xf[:, 128:256], [xf], [wbf])
                              TR(pW[:], wbf[:], idb[:], [wbf, idb], [pW])
                              wT_ = wTs[i2]
                              CP(wT_[:], pW[:], [pW], [wT_])
                              ST("sp", gd_wT[j][d][:, tsl], [R(f"gd_wT{j}{d}", b)], wT_, wT_[:])
          em.barrier()
          if stop_after <= 6:
              raise _Stop()

          with ExitStack() as es:
              hS = [sbt(es, f"hS{i}", [128, 128], F32) for i in range(4)]
              hSp = [sbt(es, f"hSp{i}", [128, 128], BF16) for i in range(4)]
              hkT = [sbt(es, f"hkT{i}", [128, 512], BF16) for i in range(4)]
              hqT = [sbt(es, f"hqT{i}", [128, 512], BF16) for i in range(4)]
              hk = [sbt(es, f"hk{i}", [64, 8, 128], BF16) for i in range(4)]
              hv = [sbt(es, f"hv{i}", [64, 8, 128], BF16) for i in range(4)]
              hos = [sbt(es, f"hos{i}", [64, 8, 128], F32) for i in range(4)]
              ham = [sbt(es, f"ham{i}", [64, 64], BF16) for i in range(4)]
              def split4(name, w):
                  t_ = pst(es, name, [128, 4, w], F32)
                  return [Buf(t_.ap[:, i, :], f"{name}{i}") for i in range(4)]
              pha = split4("pha", 64)
              pho = split4("pho", 128)
              phs = split4("phs", 128)
              gS = [sbt(es, f"gS{i}", [128, 128], F32) for i in range(4)]
              gSb = [sbt(es, f"gSb{i}", [128, 128], BF16) for i in range(4)]
              gwT = [sbt(es, f"gwT{i}", [128, 512], BF16) for i in range(4)]
              gqT = [sbt(es, f"gqT{i}", [128, 512], BF16) for i in range(4)]
              gu = [sbt(es, f"gu{i}", [128, 4, 128], F32) for i in range(4)]
              gqk = [sbt(es, f"gqk{i}", [128, 4, 128], BF16) for i in range(4)]
              gkd = [sbt(es, f"gkd{i}", [128, 4, 128], BF16) for i in range(4)]
              gos = [sbt(es, f"gos{i}", [128, 4, 128], F32) for i in range(4)]
              gvn = [sbt(es, f"gvn{i}", [128, 128], BF16) for i in range(4)]
              gob = [sbt(es, f"gob{i}", [128, 128], F32) for i in range(4)]
              pgA = split4("pgA", 128)
              pgB = split4("pgB", 128)
              pgC = split4("pgC", 128)
              pgS = split4("pgS", 128)
              for i in range(4):
                  MEMSET(hS[i][:], 0.0, [hS[i]])
                  MEMSET(hSp[i][:], 0.0, [hSp[i]])
                  MEMSET(gS[i][:], 0.0, [gS[i]])
                  MEMSET(gSb[i][:], 0.0, [gSb[i]])
              for bi in range(NB):
                  for j in range(2):
                      for d in range(2):
                          dj = d * 2 + j
                          b = bi if d == 0 else NB - 1 - bi
                          ts_ = slice(b * 512, (b + 1) * 512)
                          LD("sp", hkT[dj], hkT[dj][:], hg_khT[j][d][:, ts_], rbufs=[R(f"hg_khT{j}{d}", b)])
                          LD("sp", hqT[dj], hqT[dj][:], hg_qhT[j][d][:, ts_], rbufs=[R(f"hg_qhT{j}{d}", b)])
                          LD("sp", hk[dj], hk[dj][:], hg_kh[j][d][ts_, :].rearrange("(c p) n -> p c n", p=64), rbufs=[R(f"hg_kh{j}{d}", b)])
                          LD("sp", hv[dj], hv[dj][:], hg_v[j][ts_, :].rearrange("(c p) n -> p c n", p=64), rbufs=[R(f"hg_v{j}", b)])
                          LD("sp", gwT[dj], gwT[dj][:], gd_wT[j][d][:, ts_], rbufs=[R(f"gd_wT{j}{d}", b)])
                          LD("sp", gqT[dj], gqT[dj][:], gd_qT[j][:, ts_], rbufs=[R(f"gd_qT{j}", b)])
                          LD("sp", gu[dj], gu[dj][:], gd_u[j][d][ts_, :].rearrange("(q p) n -> p q n", p=128), rbufs=[R(f"gd_u{j}{d}", b)])
                          LD("sp", gqk[dj], gqk[dj][:], gd_qk[j][d][ts_, :].rearrange("(q p) n -> p q n", p=128), rbufs=[R(f"gd_qk{j}{d}", b)])
                          LD("sp", gkd[dj], gkd[dj][:], gd_kd[j][d][ts_, :].rearrange("(q p) n -> p q n", p=128), rbufs=[R(f"gd_kd{j}{d}", b)])
                  for step in range(8):
                      for dj in range(4):
                          d, j = dj // 2, dj % 2
                          b = bi if d == 0 else NB - 1 - bi
                          c = step if d == 0 else 7 - step
                          gch = b * 8 + c
                          nxt = gch + (1 if d == 0 else -1)
                          nxt_col = nxt if 0 <= nxt < NCH else NCH
                          cs = slice(c * 64, (c + 1) * 64)
                          kcol = C_HKF if d == 0 else C_HKB
                          em.op("pe", lambda e, dj=dj, cs=cs: e.matmul(pha[dj][0:64, :], lhsT=hkT[dj][:, cs], rhs=hqT[dj][:, cs], start=True, stop=True), [hkT[dj], hqT[dj]], [pha[dj]])
                          TT(ham[dj][:], pha[dj][0:64, :], cst[0:64, kcol:kcol + 64], ALU.mult, [pha[dj], cst], [ham[dj]])
                          em.op("pe", lambda e, dj=dj, c=c: e.matmul(pho[dj][0:64, :], lhsT=ham[dj][:], rhs=hv[dj][:, c, :], start=True, stop=False), [ham[dj], hv[dj]], [pho[dj]])
                          em.op_partial_write("pe", lambda e, dj=dj, cs=cs: e.matmul(pho[dj][0:64, :], lhsT=hqT[dj][:, cs], rhs=hSp[dj][:], start=False, stop=True), [hqT[dj], hSp[dj]], [pho[dj]])
                          em.op_partial_write("act", lambda e, dj=dj, c=c: e.activation(out=hos[dj][:, c, :], in_=pho[dj][0:64, :], func=AF.Copy), [pho[dj]], [hos[dj]])
                          em.op("pe", lambda e, dj=dj, c=c: e.matmul(phs[dj][:], lhsT=hk[dj][:, c, :], rhs=hv[dj][:, c, :], start=True, stop=True), [hk[dj], hv[dj]], [phs[dj]])
                          STT(hS[dj][:], hS[dj][:], GDEC[:, dj, gch:gch + 1], phs[dj][:], ALU.mult, ALU.add, [hS[dj], GDEC, phs[dj]], [hS[dj]])
                          TS(hSp[dj][:], hS[dj][:], GDEC[:, dj, nxt_col:nxt_col + 1], None, ALU.mult, None, [hS[dj], GDEC], [hSp[dj]], eng="dve")
                      if step % 2 == 1 and os.environ.get('NOGD') is None:
                          for dj in range(4):
                              d, j = dj // 2, dj % 2
                              b = bi if d == 0 else NB - 1 - bi
                              q4 = (step // 2) if d == 0 else 3 - (step // 2)
                              t = b * 4 + q4
                              cs = slice(q4 * 128, (q4 + 1) * 128)
                              _k = int(os.environ.get('GDK', '99'))
                              if _k > 0:
                                  em.op("pe", lambda e, dj=dj, cs=cs: e.matmul(pgA[dj][:], lhsT=gwT[dj][:, cs], rhs=gSb[dj][:], start=True, stop=True), [gwT[dj], gSb[dj]], [pgA[dj]])
                              if _k > 1:
                                  em.op("pe", lambda e, dj=dj, cs=cs: e.matmul(pgB[dj][:], lhsT=gqT[dj][:, cs], rhs=gSb[dj][:], start=True, stop=True), [gqT[dj], gSb[dj]], [pgB[dj]])
                              if _k > 2:
                                  TT(gvn[dj][:], gu[dj][:, q4, :], pgA[dj][:], ALU.subtract, [gu[dj], pgA[dj]], [gvn[dj]])
                              if _k > 3:
                                  em.op("pe", lambda e, dj=dj, q4=q4: e.matmul(pgC[dj][:], lhsT=gqk[dj][:, q4, :], rhs=gvn[dj][:], start=True, stop=True), [gqk[dj], gvn[dj]], [pgC[dj]])
                              if _k > 4:
                                  ACT(gob[dj][:], pgB[dj][:], AF.Copy, [pgB[dj], EG], [gob[dj]], scale=EG[:, dj, t:t + 1])
                              if _k > 5:
                                  em.op_partial_write("dve", lambda e, dj=dj, q4=q4: e.tensor_tensor(out=gos[dj][:, q4, :], in0=gob[dj][:], in1=pgC[dj][:], op=ALU.add), [gob[dj], pgC[dj]], [gos[dj]])
                              if _k > 6:
                                  em.op("pe", lambda e, dj=dj, q4=q4: e.matmul(pgS[dj][:], lhsT=gkd[dj][:, q4, :], rhs=gvn[dj][:], start=True, stop=True), [gkd[dj], gvn[dj]], [pgS[dj]])
                              if _k > 7:
                                  STT(gS[dj][:], gS[dj][:], GL[:, dj, t:t + 1], pgS[dj][:], ALU.mult, ALU.add, [gS[dj], GL, pgS[dj]], [gS[dj]])
                              if _k > 8:
                                  CP(gSb[dj][:], gS[dj][:], [gS[dj]], [gSb[dj]], eng="dve")
                  for j in range(2):
                      for d in range(2):
                          dj = d * 2 + j
                          b = bi if d == 0 else NB - 1 - bi
                          ts_ = slice(b * 512, (b + 1) * 512)
                          ST("sp", o_hg[j][d][ts_, :].rearrange("(c p) n -> p c n", p=64), [R(f"o_hg{j}{d}", b)], hos[dj], hos[dj][:])
                          ST("sp", o_gd[j][d][ts_, :].rearrange("(q p) n -> p q n", p=128), [R(f"o_gd{j}{d}", b)], gos[dj], gos[dj][:])
          em.barrier()
          if stop_after <= 7:
              raise _Stop()

          with ExitStack() as es:
              RT = [sbt(es, f"RT8{i}", [128, NTOK], F32) for i in range(2)]
              oa = [sbt(es, f"oa{i}", [128, 128], F32) for i in range(2)]
              ob_ = [sbt(es, f"ob8{i}", [128, 128], F32) for i in range(2)]
              osum = sbt(es, "osum", [128, 128], F32)
              junk = sbt(es, "junk8", [128, 128], F32)
              ssq = sbt(es, "ssq", [128, 2], F32)
              sz = sbt(es, "sz", [128, 128], F32)
              yy = sbt(es, "yy", [128, 128], F32)
              yT = [sbt(es, f"yT{i}", [128, 128], F32) for i in range(2)]
              pY = pst(es, "pY", [128, 128], F32)
              it = 0
              for t in range(NT):
                  rt = RT[t % 2]
                  b = t // 4
                  tsl = slice(t * 128, (t + 1) * 128)
                  LD("sp", rt, rt[:], raw_tok[tsl, :], rbufs=[R("raw_tok", b)])
                  for g in range(2):
                      for j in range(2):
                          a_, b_ = oa[it % 2], ob_[it % 2]
                          yT_ = yT[it % 2]
                          it += 1
                          src = o_hg if g == 0 else o_gd
                          nm = "o_hg" if g == 0 else "o_gd"
                          LD("sp", a_, a_[:], src[j][0][tsl, :], rbufs=[R(f"{nm}{j}0", b)])
                          LD("sp", b_, b_[:], src[j][1][tsl, :], rbufs=[R(f"{nm}{j}1", b)])
                          TT(osum[:], a_[:], b_[:], ALU.add, [a_, b_], [osum])
                          ACT(junk[:], osum[:], AF.Square, [osum], [junk, ssq], accum_out=ssq[:, 0:1])
                          ACT(ssq[:, 1:2], ssq[:, 0:1], AF.Sqrt, [ssq], [ssq], scale=1.0 / 128, bias=EPS)
                          em.op("dve", lambda e: e.reciprocal(out=ssq[:, 1:2], in_=ssq[:, 1:2]), [ssq], [ssq])
                          zc = (1024 if g == 0 else 1288) + j * 128
                          ACT(sz[:], rt[:, zc:zc + 128], AF.Silu, [rt], [sz])
                          STT(yy[:], osum[:], ssq[:, 1:2], NW[:, g * 128:(g + 1) * 128], ALU.mult, ALU.mult, [osum, ssq, NW], [yy])
                          TT(yy[:], yy[:], sz[:], ALU.mult, [yy, sz], [yy])
                          TR(pY[:], yy[:], ident, [yy, cst], [pY])
                          CP(yT_[:], pY[:], [pY], [yT_], eng="act")
                          slot = g * 2 + j
                          finals.append(em.op("sp", lambda e, slot=slot, tsl=tsl, yT_=yT_: e.dma_start(out=outT[slot * 128:(slot + 1) * 128, tsl], in_=yT_[:]),
                                              [yT_], [], dma_key=yT_.name + "_st"))
    except _Stop:
        pass
    lastk = {}
    for oid in finals:
        lastk[em.ops[oid].dma_sem] = oid
    if not finals:
        for o in em.ops:
            if o.is_dma:
                lastk[o.dma_sem] = o.id
    em.emit(final_wait_ops=list(lastk.values()))
    print("P1 ops:", em.counts, "sems:", em.n_sems)
    return nc


def prep_p1(inp, L, r):
    f32 = np.float32
    x = np.asarray(inp["x"], f32)[0]
    w_in = np.asarray(inp["w_in"], f32)[0]
    HGW = 2048
    h = [2 * r, 2 * r + 1]
    def hc(base, hh):
        return np.arange(base + hh * 128, base + (hh + 1) * 128)
    cols = []
    for base in (0, HGW, 2 * HGW, 3 * HGW, 4 * HGW):
        for hh in h:
            cols.append(hc(base, hh))
    g0 = 5 * HGW
    a0 = g0 + 6144
    b0 = a0 + 32
    z0 = b0 + 32
    ab = [a0 + d * 16 + hh for d in range(2) for hh in h] + [b0 + d * 16 + hh for d in range(2) for hh in h]
    cols.append(np.array(ab))
    for hh in h:
        cols.append(hc(z0, hh))
    tokc = np.concatenate(cols)
    assert tokc.size == NTOK
    featc = np.concatenate([hc(g0 + w * 2048, hh) for w in range(3) for hh in h])
    lb = np.asarray(inp["hg_lower_bound"], f32)
    lbp = np.stack([np.stack([np.stack([lb[l, d, hh * 128:(hh + 1) * 128] for hh in h]) for d in range(2)]) for l in range(2)])
    cw = np.asarray(inp["gd_conv_w"], f32)[0]
    convw = np.stack([cw[:, w * 2048 + hh * 128: w * 2048 + (hh + 1) * 128].T for hh in h for w in range(3)])
    al = np.asarray(inp["gd_A_log"], f32)[0]
    db = np.asarray(inp["gd_dt_bias"], f32)[0]
    gdp = np.array([al[d, hh] for d in range(2) for hh in h] + [db[d, hh] for d in range(2) for hh in h], f32)[None, :]
    nws = np.concatenate([np.asarray(inp["hg_out_norm_w"], f32)[0], np.asarray(inp["gd_out_norm_w"], f32)[0]])[None, :]
    return dict(
        xT=np.ascontiguousarray(x.T),
        w_tok=np.ascontiguousarray(w_in[:, tokc]),
        w_feat=np.ascontiguousarray(w_in[:, featc]),
        consts=make_consts(),
        anw=np.ascontiguousarray(np.asarray(inp["attn_norm_w"], f32)[0].reshape(KC, 128).T),
        lbp=np.ascontiguousarray(lbp.reshape(1, -1)),
        convw=np.ascontiguousarray(convw),
        gdp=gdp, nws=nws,
    )


import numpy as np, os
from contextlib import ExitStack
import concourse.bass as bass
import concourse.mybir as mybir

D = 4096
KC = 32
NE = 16384
EPS = 1e-6


def build_p2(TC):
    nc = bass.Bass("TRN2", target_bir_lowering=False)
    NTL = TC // 128
    dt_in = lambda n, s: nc.dram_tensor(n, s, F32, kind="ExternalInput").ap()
    x_tok = dt_in("x_tok", [TC, D])
    p_tok = dt_in("p_tok", [TC, 256])
    mixT = dt_in("mixT", [D, TC])
    w_out = dt_in("w_out", [D, D])
    w_q = dt_in("w_q", [D, 2048])
    keysT = dt_in("keysT", [128, 16, 128])
    UT = dt_in("UT", [D, NE])
    V = dt_in("V", [NE, D])
    w_gate = dt_in("w_gate", [D, D])
    w_proj = dt_in("w_proj", [256, D])
    nws = dt_in("nws", [4, D])
    ident_d = dt_in("ident", [128, 128])
    y = nc.dram_tensor("y", [TC, D], F32, kind="ExternalOutput").ap()

    em = Emitter(nc)

    def sbt(es, name, shape, dt):
        h = es.enter_context(nc.sbuf_tensor(name, shape, dt))
        return Buf(h.ap() if hasattr(h, "ap") else h, name)

    def pst(es, name, shape, dt):
        h = es.enter_context(nc.psum_tensor(name, shape, dt))
        return Buf(h.ap() if hasattr(h, "ap") else h, name)

    def ACT(out, in_, func, reads, writes, **kw):
        return em.op("act", lambda e: e.activation(out=out, in_=in_, func=func, **kw), reads, writes)

    def ACTp(out, in_, func, reads, writes, **kw):
        return em.op_partial_write("act", lambda e: e.activation(out=out, in_=in_, func=func, **kw), reads, writes)

    def MM(out, lhsT, rhs, reads, writes, start=True, stop=True):
        return em.op("pe", lambda e: e.matmul(out, lhsT=lhsT, rhs=rhs, start=start, stop=stop), reads, writes)

    def MMp(out, lhsT, rhs, reads, writes, start=True, stop=True):
        return em.op_partial_write("pe", lambda e: e.matmul(out, lhsT=lhsT, rhs=rhs, start=start, stop=stop), reads, writes)

    def TS(out, in0, s1, s2, op0, op1, reads, writes, eng="dve", partial=False):
        f = em.op_partial_write if partial else em.op
        if op1 is None:
            return f(eng, lambda e: e.tensor_scalar(out=out, in0=in0, scalar1=s1, scalar2=None, op0=op0), reads, writes)
        return f(eng, lambda e: e.tensor_scalar(out=out, in0=in0, scalar1=s1, scalar2=s2, op0=op0, op1=op1), reads, writes)

    def TT(out, in0, in1, op, reads, writes, eng="dve", partial=False):
        f = em.op_partial_write if partial else em.op
        return f(eng, lambda e: e.tensor_tensor(out=out, in0=in0, in1=in1, op=op), reads, writes)

    def STT(out, in0, scalar, in1, op0, op1, reads, writes, eng="dve", partial=False):
        f = em.op_partial_write if partial else em.op
        return f(eng, lambda e: e.scalar_tensor_tensor(out=out, in0=in0, scalar=scalar, in1=in1, op0=op0, op1=op1), reads, writes)

    def CP(out, in_, reads, writes, eng="dve", partial=False):
        f = em.op_partial_write if partial else em.op
        if eng == "act":
            return f("act", lambda e: e.activation(out=out, in_=in_, func=AF.Copy), reads, writes)
        return f(eng, lambda e: e.tensor_copy(out=out, in_=in_), reads, writes)

    def LD(eng, buf, out_ap, in_ap, partial=False):
        f = em.op_partial_write if partial else em.op
        return f(eng, lambda e: e.dma_start(out=out_ap, in_=in_ap), [], [buf], dma_key=buf.name + "_ld")

    finals = []
    with ExitStack() as es:
        idb = sbt(es, "idb", [128, 128], BF16)
        kT = sbt(es, "kT", [128, 16, 128], BF16)
        NWB = sbt(es, "NWB", [128, D], F32)
        xt = sbt(es, "xt", [128, D], F32)
        h = sbt(es, "h", [128, D], F32)
        WS = [sbt(es, "WS0", [128, KC, 512], BF16)]
        xn = sbt(es, "xn", [128, D], BF16)
        junk = xn
        xnT = sbt(es, "xnT", [128, KC, 128], BF16)
        mT = xnT
        st2 = sbt(es, "st2", [128, 4], F32)
        WQ = [sbt(es, f"WQ{i}", [128, KC, 128], BF16) for i in range(1)]
        qT = sbt(es, "qT", [128, 16, 128], BF16)
        S = sbt(es, "S", [128, 16, 128], F32)
        wk = sbt(es, "wk", [128, 256], F32)
        t1 = sbt(es, "t1", [128, 16], F32)
        t2 = sbt(es, "t2", [128, 16], F32)
        cand = sbt(es, "cand", [128, 16, 16], F32)
        ct = sbt(es, "ct", [128, 16], F32)
        ez = sbt(es, "ez", [128, 16], F32)
        sc = sbt(es, "sc", [128, 8], F32)
        E1Z = sbt(es, "E1Z", [128, 8, 128], F32)
        E2 = sbt(es, "E2", [128, 8, 128], F32)
        TH = sbt(es, "TH", [128, 8, 128], F32)
        gm = [sbt(es, f"gm{i}", [128, 128], F32) for i in range(2)]
        gh = [sbt(es, f"gh{i}", [128, 128], BF16) for i in range(2)]
        UTt = [sbt(es, f"UTt{i}", [128, KC, 128], BF16) for i in range(2)]
        gel = [sbt(es, f"gel{i}", [128, 128], F32) for i in range(2)]
        WT = sbt(es, "WT", [128, 128, 128], BF16)
        pt = sbt(es, "pt", [128, 256], F32)
        ptb = sbt(es, "ptb", [128, 256], BF16)
        pTT = sbt(es, "pTT", [128, 2, 128], BF16)
        WP = sbt(es, "WP", [128, 2, D], BF16)
        emb = xt
        gate = sbt(es, "gate", [128, 512], F32)
        pA = [pst(es, f"pA{i}", [128, 512], F32) for i in range(2)]
        pTr = pst(es, "pTr", [128, 4, 128], BF16)
        pQ = [pst(es, "pQ0", [128, 128], F32)] * 2
        pG = [pst(es, f"pG{i}", [128, 128], F32) for i in range(2)]
        pH = [pst(es, f"pH{i}", [128, 128], F32) for i in range(2)]

        LD("pool", idb, idb[:], ident_d[:, :])
        LD("pool", kT, kT[:], keysT[:, :, :])
        LD("pool", WP, WP[:], w_proj.rearrange("(c p) n -> p c n", p=128))
        def dramb(name, shape):
            return nc.dram_tensor(name, shape, BF16, kind="Internal").ap()
        w_out_b, w_q_b, w_gate_b, UT_b, V_b = dramb("w_out_b", [D, D]), dramb("w_q_b", [D, 2048]), dramb("w_gate_b", [D, D]), dramb("UT_b", [D, NE]), dramb("V_b", [NE, D])
        pcb = Buf(None, "precast")
        npc = [0]
        for (srcw, dstw, rows, cols) in ((w_out, w_out_b, D, D), (w_q, w_q_b, D, 2048), (UT, UT_b, D, NE), (V, V_b, NE, D), (w_gate, w_gate_b, D, D)):
            for r_ in range(0, rows, 128):
                for c_ in range(0, cols, 4096):
                    c1 = min(cols, c_ + 4096)
                    em.op_partial_write("pool", lambda e, dstw=dstw, srcw=srcw, r_=r_, c_=c_, c1=c1: e.dma_start(out=dstw[r_:r_ + 128, c_:c1], in_=srcw[r_:r_ + 128, c_:c1]),
                                        [], [pcb], dma_key=f"pc{npc[0] % 4}")
                    npc[0] += 1
        em.barrier()
        wo3 = w_out_b.rearrange("(kc p) n -> p kc n", p=128)
        wq3 = w_q_b.rearrange("(kc p) n -> p kc n", p=128)
        wg3 = w_gate_b.rearrange("(kc p) n -> p kc n", p=128)
        ut3 = UT_b.rearrange("(kc p) n -> p kc n", p=128)
        v3 = V_b.rearrange("(i p) n -> p i n", p=128)
        mx3 = mixT.rearrange("(kc p) t -> p kc t", p=128)
        wsi = [0]

        def rms_to_xn(src, nrow):
            LD("sp", NWB, NWB[:], nws[nrow:nrow + 1, :].partition_broadcast(128))
            ACT(junk[:], src[:], AF.Square, [src], [junk, st2], accum_out=st2[:, 0:1])
            ACT(st2[:, 1:2], st2[:, 0:1], AF.Sqrt, [st2], [st2], scale=1.0 / D, bias=EPS)
            em.op("dve", lambda e: e.reciprocal(out=st2[:, 1:2], in_=st2[:, 1:2]), [st2], [st2])
            STT(xn[:], src[:], st2[:, 1:2], NWB[:], ALU.mult, ALU.mult, [src, st2, NWB], [xn])
            for g in range(8):
                for q in range(4):
                    kc = g * 4 + q
                    f = em.op if q == 0 else em.op_partial_write
                    f("pe", lambda e, kc=kc, q=q: e.transpose(pTr[:, q, :], xn[:, kc * 128:(kc + 1) * 128], idb[:]), [xn, idb], [pTr])
                CP(xnT[:, g * 4:(g + 1) * 4, :], pTr[:], [pTr], [xnT], eng=("act" if g % 2 else "dve"), partial=(g > 0))

        def stream_w(w3, c0, width=512):
            ws = WS[0]
            wsi[0] += 1
            for g in range(4):
                LD("sp", ws, ws[:, g * 8:(g + 1) * 8, 0:width], w3[:, g * 8:(g + 1) * 8, c0:c0 + width], partial=(g > 0))
            return ws

        for tl in range(NTL):
            r0 = tl * 128
            rs_ = slice(r0, r0 + 128)
            LD("sp", xt, xt[:], x_tok[rs_, :])
            LD("pool", mT, mT[:], mx3[:, :, rs_])
            for cg in range(8):
                ws = stream_w(wo3, cg * 512)
                ps = pA[cg % 2]
                for kc in range(KC):
                    MM(ps[:], mT[:, kc, :], ws[:, kc, :], [mT, ws], [ps], start=(kc == 0), stop=(kc == KC - 1))
                TT(h[:, cg * 512:(cg + 1) * 512], xt[:, cg * 512:(cg + 1) * 512], ps[:], ALU.add, [xt, ps], [h], partial=(cg > 0))
            rms_to_xn(h, 0)
            for hp in range(16):
                wq = WQ[0]
                for g in range(4):
                    LD("sp", wq, wq[:, g * 8:(g + 1) * 8, :], wq3[:, g * 8:(g + 1) * 8, hp * 128:(hp + 1) * 128], partial=(g > 0))
                pq = pQ[hp % 2]
                for kc in range(KC):
                    MM(pq[:], wq[:, kc, :], xnT[:, kc, :], [wq, xnT], [pq], start=(kc == 0), stop=(kc == KC - 1))
                CP(qT[:, hp, :], pq[:], [pq], [qT], eng="act", partial=(hp > 0))
            for hp in range(16):
                pq = pQ[hp % 2]
                MM(pq[:], qT[:, hp, :], kT[:, hp, :], [qT, kT], [pq])
                CP(S[:, hp, :], pq[:], [pq], [S], eng=("act" if hp % 2 else "dve"), partial=(hp > 0))
            for hh in range(8):
                s1 = S[:, 2 * hh, :]
                s2 = S[:, 2 * hh + 1, :]
                for (src, tt_) in ((s1, t1), (s2, t2)):
                    em.op("dve", lambda e, src=src, tt_=tt_: e.max(out=tt_[:, 0:8], in_=src), [S], [tt_])
                    em.op("dve", lambda e, src=src, tt_=tt_: e.match_replace(out=wk[:, 0:128], in_to_replace=tt_[:, 0:8], in_values=src, imm_value=-1e30), [S, tt_], [wk])
                    em.op_partial_write("dve", lambda e, tt_=tt_: e.max(out=tt_[:, 8:16], in_=wk[:, 0:128]), [wk], [tt_])
                for a in range(16):
                    TS(cand[:, a, :], t2[:], t1[:, a:a + 1], None, ALU.add, None, [t1, t2], [cand], partial=(a > 0))
                cflat = cand[:].rearrange("p a b -> p (a b)")
                em.op("dve", lambda e, cflat=cflat: e.max(out=ct[:, 0:8], in_=cflat), [cand], [ct])
                em.op("dve", lambda e, cflat=cflat: e.match_replace(out=wk[:], in_to_replace=ct[:, 0:8], in_values=cflat, imm_value=-1e30), [cand, ct], [wk])
                em.op_partial_write("dve", lambda e: e.max(out=ct[:, 8:16], in_=wk[:]), [wk], [ct])
                TS(sc[:, 0:1], ct[:, 0:1], -1.0, None, ALU.mult, None, [ct], [sc])
                ACT(ez[:], ct[:], AF.Exp, [ct, sc], [ez, sc], bias=sc[:, 0:1], accum_out=sc[:, 1:2])
                em.op("dve", lambda e: e.reciprocal(out=sc[:, 2:3], in_=sc[:, 1:2]), [sc], [sc])
                TS(sc[:, 3:4], t1[:, 0:1], -1.0, None, ALU.mult, None, [t1, sc], [sc])
                TS(sc[:, 4:5], t2[:, 0:1], -1.0, None, ALU.mult, None, [t2, sc], [sc])
                ACTp(E1Z[:, hh, :], s1, AF.Exp, [S, sc], [E1Z], bias=sc[:, 3:4])
                TS(E1Z[:, hh, :], E1Z[:, hh, :], sc[:, 2:3], None, ALU.mult, None, [E1Z, sc], [E1Z], partial=True)
                ACTp(E2[:, hh, :], s2, AF.Exp, [S, sc], [E2], bias=sc[:, 4:5])
                TS(TH[:, hh, :], s1, -1.0, ct[:, 15:16], ALU.mult, ALU.add, [S, ct], [TH], partial=True)
            it = 0
            for i in range(128):
                ut = UTt[i % 2]
                for g in range(4):
                    LD("sp", ut, ut[:, g * 8:(g + 1) * 8, :], ut3[:, g * 8:(g + 1) * 8, i * 128:(i + 1) * 128], partial=(g > 0))
                pg, ph = pG[i % 2], pH[i % 2]
                for hh in range(8):
                    g_, gh_ = gm[it % 2], gh[it % 2]
                    it += 1
                    STT(g_[:], S[:, 2 * hh + 1, :], TH[:, hh, i:i + 1], E2[:, hh, :], ALU.is_ge, ALU.mult, [S, TH, E2], [g_])
                    ACT(gh_[:], g_[:], AF.Copy, [g_, E1Z], [gh_], scale=E1Z[:, hh, i:i + 1])
                    f = MM if hh == 0 else MMp
                    f(pg[:], gh_[:], idb[:], [gh_, idb], [pg], start=(hh == 0), stop=(hh == 7))
                for kc in range(KC):
                    f = MM if kc == 0 else MMp
                    f(ph[:], ut[:, kc, :], xnT[:, kc, :], [ut, xnT], [ph], start=(kc == 0), stop=(kc == KC - 1))
                ge = gel[i % 2]
                ACT(ge[:], ph[:], AF.Gelu, [ph], [ge])
                TT(WT[:, i, :], ge[:], pg[:], ALU.mult, [ge, pg], [WT], partial=(i > 0))
            for cg in range(8):
                ps = pA[cg % 2]
                for ic in range(8):
                    vc = WS[0]
                    LD("sp", vc, vc[:, 0:16, :], v3[:, ic * 16:(ic + 1) * 16, cg * 512:(cg + 1) * 512])
                    for q in range(16):
                        i = ic * 16 + q
                        f = MM if i == 0 else MMp
                        f(ps[:], WT[:, i, :], vc[:, q, :], [WT, vc], [ps], start=(i == 0), stop=(i == 127))
                TT(h[:, cg * 512:(cg + 1) * 512], h[:, cg * 512:(cg + 1) * 512], ps[:], ALU.add, [h, ps], [h], partial=True)
            rms_to_xn(h, 1)
            LD("sp", pt, pt[:], p_tok[rs_, :])
            CP(ptb[:], pt[:], [pt], [ptb])
            for q in range(2):
                f = em.op if q == 0 else em.op_partial_write
                f("pe", lambda e, q=q: e.transpose(pTr[:, q, :], ptb[:, q * 128:(q + 1) * 128], idb[:]), [ptb, idb], [pTr])
            CP(pTT[:], pTr[:, 0:2, :], [pTr], [pTT])
            for cg in range(8):
                ps = pA[cg % 2]
                for q in range(2):
                    f = MM if q == 0 else MMp
                    f(ps[:], pTT[:, q, :], WP[:, q, cg * 512:(cg + 1) * 512], [pTT, WP], [ps], start=(q == 0), stop=(q == 1))
                CP(emb[:, cg * 512:(cg + 1) * 512], ps[:], [ps], [emb], eng="act", partial=(cg > 0))
            LD("sp", NWB, NWB[:], nws[2:3, :].partition_broadcast(128))
            ACT(junk[:], emb[:], AF.Square, [emb], [junk, st2], accum_out=st2[:, 2:3])
            ACT(st2[:, 3:4], st2[:, 2:3], AF.Sqrt, [st2], [st2], scale=1.0 / D, bias=EPS)
            em.op("dve", lambda e: e.reciprocal(out=st2[:, 3:4], in_=st2[:, 3:4]), [st2], [st2])
            STT(emb[:], emb[:], st2[:, 3:4], NWB[:], ALU.mult, ALU.mult, [emb, st2, NWB], [emb])
            for cg in range(8):
                ws = stream_w(wg3, cg * 512)
                ps = pA[cg % 2]
                for kc in range(KC):
                    f = MM if kc == 0 else MMp
                    f(ps[:], xnT[:, kc, :], ws[:, kc, :], [xnT, ws], [ps], start=(kc == 0), stop=(kc == KC - 1))
                ACT(gate[:], ps[:], AF.Sigmoid, [ps], [gate])
                TT(gate[:], gate[:], emb[:, cg * 512:(cg + 1) * 512], ALU.mult, [gate, emb], [gate])
                TT(h[:, cg * 512:(cg + 1) * 512], h[:, cg * 512:(cg + 1) * 512], gate[:], ALU.add, [h, gate], [h], partial=True)
            LD("sp", NWB, NWB[:], nws[3:4, :].partition_broadcast(128))
            ACT(junk[:], h[:], AF.Square, [h], [junk, st2], accum_out=st2[:, 0:1])
            ACT(st2[:, 1:2], st2[:, 0:1], AF.Sqrt, [st2], [st2], scale=1.0 / D, bias=EPS)
            em.op("dve", lambda e: e.reciprocal(out=st2[:, 1:2], in_=st2[:, 1:2]), [st2], [st2])
            STT(xt[:], h[:], st2[:, 1:2], NWB[:], ALU.mult, ALU.mult, [h, st2, NWB], [xt])
            finals.append(em.op("sp", lambda e, rs_=rs_: e.dma_start(out=y[rs_, :], in_=xt[:]), [xt], [], dma_key="xt_st"))
    em.emit(final_wait_ops=[finals[-1]])
    print("P2 ops:", em.counts, "sems:", em.n_sems)
    return nc


def prep_p2(inp, mix_all, L, r):
    f32 = np.float32
    TC = L // 8
    ts = slice(r * TC, (r + 1) * TC)
    perm = []
    for c in range(8):
        for g in range(2):
            for j in range(2):
                hh = 2 * c + j
                perm.append(np.arange(g * 2048 + hh * 128, g * 2048 + (hh + 1) * 128))
    perm = np.concatenate(perm)
    sk = np.asarray(inp["peer_sub_keys"], f32)[0]
    keysT = np.ascontiguousarray(sk.reshape(16, 128, 128).transpose(2, 0, 1))
    nws = np.stack([np.asarray(inp[k], f32).reshape(-1) for k in ("ffn_norm_w", "ple_norm_w", "ple_post_norm_w", "final_norm_w")])
    return dict(
        x_tok=np.ascontiguousarray(np.asarray(inp["x"], f32)[0, ts]),
        p_tok=np.ascontiguousarray(np.asarray(inp["p"], f32)[0, 0, ts]),
        mixT=np.ascontiguousarray(mix_all[:, ts]),
        w_out=np.ascontiguousarray(np.asarray(inp["w_out"], f32)[0][perm]),
        w_q=np.asarray(inp["peer_w_query"], f32)[0],
        keysT=keysT,
        UT=np.ascontiguousarray(np.asarray(inp["peer_u"], f32)[0].T),
        V=np.asarray(inp["peer_v"], f32)[0],
        w_gate=np.asarray(inp["ple_w_gate"], f32)[0],
        w_proj=np.asarray(inp["ple_w_proj"], f32)[0],
        nws=np.ascontiguousarray(nws),
        ident=np.eye(128, dtype=f32),
    )


def kernel(**inputs):
    from concourse.bass_utils import run_bass_kernel_spmd
    L = int(np.asarray(inputs["x"]).shape[1])
    nc1 = build_p1(L)
    maps1 = [prep_p1(inputs, L, r) for r in range(8)]
    res1 = run_bass_kernel_spmd(nc1, maps1, core_ids=list(range(8)))
    mix_all = np.concatenate([res1.results[r]["outT"] for r in range(8)], axis=0)
    del maps1
    nc2 = build_p2(L // 8)
    maps2 = [prep_p2(inputs, mix_all, L, r) for r in range(8)]
    res2 = run_bass_kernel_spmd(nc2, maps2, core_ids=list(range(8)))
    out = np.concatenate([res2.results[r]["y"] for r in range(8)], axis=0)
    return out.reshape(1, L, 4096).astype(np.float32)
```
